# Optimizing a Trainium2 kernel written in Bass

```python
import jax
import jax.numpy as jnp
from jax import lax
import numpy as np

D_MODEL = 1024
BATCH = 8
SEQ = 4096
DEPTH = 2

GRID_W = 64
CTX_LEN = 256

CONV_DIM = 256
ATT_HEADS = 8
ATT_KV_HEADS = 2
ATT_HEAD_DIM = 64
ATT_WINDOW = 128
ATT_BLOCK = 128
ROPE_BASE = 10000.0
MLSTM_HEADS = 4
MLSTM_HEAD_DIM = 64
MLSTM_DIM = MLSTM_HEADS * MLSTM_HEAD_DIM
MLSTM_CHUNK = 64
N_BRANCHES = 3
N_GROUPS = 4
EXPERTS_PER_GROUP = 8
N_EXPERTS = N_GROUPS * EXPERTS_PER_GROUP
TOP_K = 2
D_EXPERT = D_MODEL // 2
EXPERT_BLOCK = 128
DEEPNORM_ALPHA = (2 * DEPTH) ** 0.25
DEEPNORM_BETA = (8 * DEPTH) ** -0.25
LN_EPS = 1e-6
NEG_INF = -1e30

SPLIT_SIZES = (
    CONV_DIM, CONV_DIM, CONV_DIM,
    ATT_HEADS * ATT_HEAD_DIM,
    ATT_KV_HEADS * ATT_HEAD_DIM,
    ATT_KV_HEADS * ATT_HEAD_DIM,
    MLSTM_DIM, MLSTM_DIM, MLSTM_DIM, MLSTM_DIM,
    4 * MLSTM_HEADS,
    N_BRANCHES * D_MODEL,
)
IN_COLS = sum(SPLIT_SIZES)

kernel_name = 'hybrid_flow_block'


def _layer_norm(x, gain=None, bias=None):
    xf = x.astype(jnp.float32)
    mu = jnp.mean(xf, axis=-1, keepdims=True)
    var = jnp.mean(jnp.square(xf - mu), axis=-1, keepdims=True)
    y = ((xf - mu) * lax.rsqrt(var + LN_EPS)).astype(x.dtype)
    if gain is not None:
        y = y * gain + bias
    return y


def _modulate(h, shift, scale):
    return h * (1 + scale) + shift


def _split_cols(z):
    offs = np.cumsum(SPLIT_SIZES)[:-1].tolist()
    return jnp.split(z, offs, axis=-1)


def _short_conv(u, w):
    up = jnp.pad(u, ((0, 0), (1, 1), (0, 0)))
    return up[:, :-2] * w[0] + up[:, 1:-1] * w[1] + up[:, 2:] * w[2]


def _rope_tables(n_tokens):
    rows = n_tokens // GRID_W
    pos_r = jnp.repeat(jnp.arange(rows, dtype=jnp.float32), GRID_W)
    pos_c = jnp.tile(jnp.arange(GRID_W, dtype=jnp.float32), rows)
    nf = ATT_HEAD_DIM // 4
    inv = ROPE_BASE ** (-jnp.arange(nf, dtype=jnp.float32) / nf)
    ang_r = pos_r[:, None] * inv
    ang_c = pos_c[:, None] * inv
    return (jnp.cos(ang_r), jnp.sin(ang_r), jnp.cos(ang_c), jnp.sin(ang_c))


def _rotate_half(x, cos, sin):
    x1, x2 = jnp.split(x, 2, axis=-1)
    cos = cos[None, :, None, :].astype(x.dtype)
    sin = sin[None, :, None, :].astype(x.dtype)
    return jnp.concatenate([x1 * cos - x2 * sin, x2 * cos + x1 * sin], axis=-1)


def _rope_2d(x, tabs):
    cr, sr, cc, sc = tabs
    xr, xc = jnp.split(x, 2, axis=-1)
    return jnp.concatenate([_rotate_half(xr, cr, sr), _rotate_half(xc, cc, sc)], axis=-1)


def _window_attention(q, k, v, kc, vc, sink):
    B, L, H, dh = q.shape
    G = H // ATT_KV_HEADS
    nb = L // ATT_BLOCK
    n_ctx = kc.shape[1]
    scale = dh ** -0.5
    qb = q.reshape(B, nb, ATT_BLOCK, ATT_KV_HEADS, G, dh)
    pad = ((0, 0), (ATT_BLOCK, ATT_BLOCK), (0, 0), (0, 0))
    kp = jnp.pad(k, pad).reshape(B, nb + 2, ATT_BLOCK, ATT_KV_HEADS, dh)
    vp = jnp.pad(v, pad).reshape(B, nb + 2, ATT_BLOCK, ATT_KV_HEADS, dh)
    kw = jnp.concatenate([kp[:, :-2], kp[:, 1:-1], kp[:, 2:]], axis=2)
    vw = jnp.concatenate([vp[:, :-2], vp[:, 1:-1], vp[:, 2:]], axis=2)
    sink_l = jnp.broadcast_to(
        sink.astype(jnp.float32).reshape(ATT_KV_HEADS, G)[None, :, :, None, None],
        (B, ATT_KV_HEADS, G, ATT_BLOCK, 1))
    offs_q = jnp.arange(ATT_BLOCK)
    offs_k = jnp.arange(3 * ATT_BLOCK) - ATT_BLOCK

    def block(args):
        qi, ki, vi, bi = args
        qpos = bi * ATT_BLOCK + offs_q
        kpos = bi * ATT_BLOCK + offs_k
        valid = ((jnp.abs(qpos[:, None] - kpos[None, :]) <= ATT_WINDOW)
                 & (kpos >= 0)[None, :] & (kpos < L)[None, :])
        s_w = jnp.einsum('bqkgd,bskd->bkgqs', qi, ki, preferred_element_type=jnp.float32) * scale
        s_w = jnp.where(valid, s_w, NEG_INF)
        s_c = jnp.einsum('bqkgd,bskd->bkgqs', qi, kc, preferred_element_type=jnp.float32) * scale
        p = jax.nn.softmax(jnp.concatenate([s_c, s_w, sink_l], axis=-1), axis=-1)
        o = (jnp.einsum('bkgqs,bskd->bqkgd', p[..., :n_ctx].astype(vc.dtype), vc)
             + jnp.einsum('bkgqs,bskd->bqkgd', p[..., n_ctx:-1].astype(vi.dtype), vi))
        return o

    xs = (jnp.moveaxis(qb, 1, 0), jnp.moveaxis(kw, 1, 0), jnp.moveaxis(vw, 1, 0), jnp.arange(nb))
    o = lax.map(block, xs)
    return jnp.moveaxis(o, 0, 1).reshape(B, L, H * dh)


def _context_attention(q, k, v, sink):
    B, C, H, dh = q.shape
    G = H // ATT_KV_HEADS
    qg = q.reshape(B, C, ATT_KV_HEADS, G, dh)
    s = jnp.einsum('bqkgd,bskd->bkgqs', qg, k, preferred_element_type=jnp.float32) * dh ** -0.5
    s_sink = jnp.broadcast_to(
        sink.astype(jnp.float32).reshape(ATT_KV_HEADS, G)[None, :, :, None, None],
        (B, ATT_KV_HEADS, G, C, 1))
    p = jax.nn.softmax(jnp.concatenate([s, s_sink], axis=-1), axis=-1)[..., :-1]
    o = jnp.einsum('bkgqs,bskd->bqkgd', p.astype(v.dtype), v)
    return o.reshape(B, C, H * dh)


def _mlstm_chunkwise(q, k, v, li, lf, skip_chunks):
    B, T, H, dh = q.shape
    nc = T // MLSTM_CHUNK
    f32 = jnp.float32

    def chunks(t):
        return jnp.moveaxis(t.astype(f32).reshape(B, nc, MLSTM_CHUNK, H, -1), 3, 1)

    def gchunks(t):
        return jnp.moveaxis(t.astype(f32).reshape(B, nc, MLSTM_CHUNK, H), 3, 1)

    q, k, v = chunks(q), chunks(k) * dh ** -0.5, chunks(v)
    li, lf = gchunks(li), gchunks(lf)
    bcum = jnp.cumsum(lf, axis=-1)
    g = bcum[..., -1]
    w_loc = g[..., None] - bcum + li
    m_loc = jnp.max(w_loc, axis=-1)
    e_loc = jnp.exp(w_loc - m_loc[..., None])
    c_loc = jnp.einsum('bhncd,bhnce->bhnde', k * e_loc[..., None], v)
    n_loc = jnp.einsum('bhncd,bhnc->bhnd', k, e_loc)

    def step(carry, inp):
        c_st, n_st, m_st = carry
        g_j, c_j, n_j, m_j = inp
        m_new = jnp.maximum(g_j + m_st, m_j)
        a = jnp.exp(g_j + m_st - m_new)
        b = jnp.exp(m_j - m_new)
        c_new = a[..., None, None] * c_st + b[..., None, None] * c_j
        n_new = a[..., None] * n_st + b[..., None] * n_j
        return (c_new, n_new, m_new), (c_st, n_st, m_st)

    init = (jnp.zeros((B, H, dh, dh), f32), jnp.zeros((B, H, dh), f32), jnp.zeros((B, H), f32))
    xs = (jnp.moveaxis(g, 2, 0), jnp.moveaxis(c_loc, 2, 0), jnp.moveaxis(n_loc, 2, 0), jnp.moveaxis(m_loc, 2, 0))
    _, (c_prev, n_prev, m_prev) = lax.scan(step, init, xs)
    c_prev = jnp.moveaxis(c_prev, 0, 2)
    n_prev = jnp.moveaxis(n_prev, 0, 2)
    m_prev = jnp.moveaxis(m_prev, 0, 2)

    s = skip_chunks
    q, k, v, li, bcum = q[:, :, s:], k[:, :, s:], v[:, :, s:], li[:, :, s:], bcum[:, :, s:]
    c_prev, n_prev, m_prev = c_prev[:, :, s:], n_prev[:, :, s:], m_prev[:, :, s:]

    a = bcum + m_prev[..., None]
    dmat = bcum[..., :, None] - bcum[..., None, :] + li[..., None, :]
    lower = jnp.tril(jnp.ones((MLSTM_CHUNK, MLSTM_CHUNK), dtype=bool))
    dmat = jnp.where(lower, dmat, -jnp.inf)
    m = jnp.maximum(a, jnp.max(dmat, axis=-1))
    w_intra = jnp.exp(dmat - m[..., None])
    e_inter = jnp.exp(a - m)
    s_qk = jnp.einsum('bhnsd,bhnrd->bhnsr', q, k) * w_intra
    num = (jnp.einsum('bhnsr,bhnre->bhnse', s_qk, v)
           + e_inter[..., None] * jnp.einsum('bhnsd,bhnde->bhnse', q, c_prev))
    den = jnp.sum(s_qk, axis=-1) + e_inter * jnp.einsum('bhnsd,bhnd->bhns', q, n_prev)
    h = num / jnp.maximum(jnp.abs(den), jnp.exp(-m))[..., None]
    return jnp.moveaxis(h, 1, 3).reshape(B, -1, H, dh)


def _head_norm(h, w):
    hf = h.astype(jnp.float32)
    mu = jnp.mean(hf, axis=-1, keepdims=True)
    var = jnp.mean(jnp.square(hf - mu), axis=-1, keepdims=True)
    y = (hf - mu) * lax.rsqrt(var + LN_EPS)
    return y.reshape(h.shape[0], h.shape[1], -1) * w.astype(jnp.float32)


def _token_mixer(hl, hc, w_in, conv_w, attn_sink, gate_b, norm_w, w_pa, w_pb, w_pc, w_o, rope, need_ctx):
    B, L, _ = hl.shape
    n_ctx = hc.shape[1]
    zl = _split_cols(hl @ w_in)
    zc = _split_cols(hc @ w_in)

    def conv_mixer(z):
        return z[0] * _short_conv(z[1] * z[2], conv_w)

    ql = _rope_2d(zl[3].reshape(B, L, ATT_HEADS, ATT_HEAD_DIM), rope)
    kl = _rope_2d(zl[4].reshape(B, L, ATT_KV_HEADS, ATT_HEAD_DIM), rope)
    vl = zl[5].reshape(B, L, ATT_KV_HEADS, ATT_HEAD_DIM)
    qc = zc[3].reshape(B, n_ctx, ATT_HEADS, ATT_HEAD_DIM)
    kc = zc[4].reshape(B, n_ctx, ATT_KV_HEADS, ATT_HEAD_DIM)
    vc = zc[5].reshape(B, n_ctx, ATT_KV_HEADS, ATT_HEAD_DIM)

    def heads(t):
        return t.reshape(t.shape[0], t.shape[1], MLSTM_HEADS, MLSTM_HEAD_DIM)

    def seq(tc, tl, rev):
        if rev:
            return jnp.concatenate([jnp.flip(tc, axis=1), jnp.flip(tl, axis=1)], axis=1)
        return jnp.concatenate([tc, tl], axis=1)

    gates_l = zl[10].astype(jnp.float32) + gate_b.astype(jnp.float32)
    gates_c = zc[10].astype(jnp.float32) + gate_b.astype(jnp.float32)
    skip = 0 if need_ctx else n_ctx // MLSTM_CHUNK
    outs = []
    for d, rev in enumerate((False, True)):
        i_sl = slice(2 * d * MLSTM_HEADS, (2 * d + 1) * MLSTM_HEADS)
        f_sl = slice((2 * d + 1) * MLSTM_HEADS, (2 * d + 2) * MLSTM_HEADS)
        q_d = seq(heads(zc[6]), heads(zl[6]), rev)
        k_d = seq(heads(zc[7]), heads(zl[7]), rev)
        v_d = seq(heads(zc[8]), heads(zl[8]), rev)
        li_d = seq(gates_c[..., i_sl], gates_l[..., i_sl], rev)
        lf_d = jax.nn.log_sigmoid(seq(gates_c[..., f_sl], gates_l[..., f_sl], rev))
        outs.append(_mlstm_chunkwise(q_d, k_d, v_d, li_d, lf_d, skip))
    h_fwd, h_bwd = outs
    hm_l = h_fwd[:, -L:] + jnp.flip(h_bwd[:, -L:], axis=1)
    ym_l = jax.nn.sigmoid(zl[9]) * _head_norm(hm_l, norm_w).astype(hl.dtype)

    def merge(ya, yb, yc, g):
        g_a, g_b, g_c = jnp.split(jax.nn.sigmoid(g), N_BRANCHES, axis=-1)
        return (g_a * (ya @ w_pa) + g_b * (yb @ w_pb) + g_c * (yc @ w_pc)) @ w_o

    yl = merge(conv_mixer(zl), _window_attention(ql, kl, vl, kc, vc, attn_sink), ym_l, zl[11])
    if not need_ctx:
        return yl, None
    hm_c = h_fwd[:, :n_ctx] + jnp.flip(h_bwd[:, :n_ctx], axis=1)
    ym_c = jax.nn.sigmoid(zc[9]) * _head_norm(hm_c, norm_w).astype(hc.dtype)
    yc = merge(conv_mixer(zc), _context_attention(qc, kc, vc, attn_sink), ym_c, zc[11])
    return yl, yc


def _hier_moe(h, w_rg, b_rg, w_re, b_re, w_ei, w_eo):
    N, D = h.shape
    A = N * TOP_K
    P = -(-(A + N_EXPERTS * EXPERT_BLOCK) // EXPERT_BLOCK) * EXPERT_BLOCK
    NB = P // EXPERT_BLOCK
    rows = jnp.arange(N)
    g_logit = (h @ w_rg + b_rg).astype(jnp.float32)
    g_prob = jax.nn.softmax(g_logit, axis=-1)
    g_sel = jnp.argmax(g_logit, axis=-1).astype(jnp.int32)
    e_logit = (h @ w_re + b_re).astype(jnp.float32).reshape(N, N_GROUPS, EXPERTS_PER_GROUP)[rows, g_sel]
    top_l, top_i = lax.top_k(e_logit, TOP_K)
    wts = (jax.nn.softmax(top_l, axis=-1) * g_prob[rows, g_sel][:, None]).reshape(A)
    eid = (g_sel[:, None] * EXPERTS_PER_GROUP + top_i).reshape(A)
    tok = jnp.repeat(rows, TOP_K)
    order = jnp.argsort(eid)
    eid_s, tok_s, w_s = eid[order], tok[order], wts[order]
    counts = jnp.bincount(eid, length=N_EXPERTS)
    padded = (counts + EXPERT_BLOCK - 1) // EXPERT_BLOCK * EXPERT_BLOCK
    pad_end = jnp.cumsum(padded)
    pad_start = pad_end - padded
    start = jnp.cumsum(counts) - counts
    pos = pad_start[eid_s] + jnp.arange(A) - start[eid_s]
    buf_tok = jnp.full((P,), N, dtype=jnp.int32).at[pos].set(tok_s)
    buf_w = jnp.zeros((P,), h.dtype).at[pos].set(w_s.astype(h.dtype))
    blk_e = jnp.minimum(jnp.searchsorted(pad_end, jnp.arange(NB) * EXPERT_BLOCK, side='right'), N_EXPERTS - 1)
    h_pad = jnp.concatenate([h, jnp.zeros((1, D), h.dtype)], axis=0)
    xb = h_pad[buf_tok].reshape(NB, EXPERT_BLOCK, D)

    def expert_block(args):
        xblk, e = args
        gt, up = jnp.split(xblk @ w_ei[e], 2, axis=-1)
        return (jax.nn.silu(gt) * up) @ w_eo[e]

    yb = lax.map(expert_block, (xb, blk_e)).reshape(P, D)
    out = jax.ops.segment_sum(yb * buf_w[:, None], buf_tok, num_segments=N + 1)
    return out[:N]


def setup_inputs(seed: int = 0) -> dict:
    key = jax.random.key(seed)
    ks = jax.random.split(key, 26)

    def nrm(k, shape):
        return jax.random.normal(k, shape, jnp.float32)

    D = D_MODEL
    gate_offset = jnp.concatenate([
        jnp.zeros((MLSTM_HEADS,), jnp.float32), jnp.linspace(3.0, 6.0, MLSTM_HEADS, dtype=jnp.float32),
        jnp.zeros((MLSTM_HEADS,), jnp.float32), jnp.linspace(3.0, 6.0, MLSTM_HEADS, dtype=jnp.float32)])
    return {
        'x': nrm(ks[0], (BATCH, SEQ, D)),
        'c': nrm(ks[1], (BATCH, D)),
        'ctx': nrm(ks[2], (BATCH, CTX_LEN, D)),
        'c_ctx': nrm(ks[3], (D,)),
        'w_ada': nrm(ks[4], (DEPTH, D, 6 * D)) * (0.5 * D ** -0.5),
        'b_ada': 0.01 * nrm(ks[5], (DEPTH, 6 * D)),
        'w_in': nrm(ks[6], (DEPTH, D, IN_COLS)) * D ** -0.5,
        'conv_w': nrm(ks[7], (DEPTH, 3, CONV_DIM)) * 3 ** -0.5,
        'attn_sink': 0.5 * nrm(ks[8], (DEPTH, ATT_HEADS)),
        'mlstm_gate_b': gate_offset[None, :] + 0.1 * nrm(ks[9], (DEPTH, 4 * MLSTM_HEADS)),
        'mlstm_norm_w': 1.0 + 0.02 * nrm(ks[10], (DEPTH, MLSTM_DIM)),
        'w_proj_a': nrm(ks[11], (DEPTH, CONV_DIM, D)) * (CONV_DIM ** -0.5 * DEEPNORM_BETA),
        'w_proj_b': nrm(ks[12], (DEPTH, ATT_HEADS * ATT_HEAD_DIM, D)) * ((ATT_HEADS * ATT_HEAD_DIM) ** -0.5 * DEEPNORM_BETA),
        'w_proj_c': nrm(ks[13], (DEPTH, MLSTM_DIM, D)) * (MLSTM_DIM ** -0.5 * DEEPNORM_BETA),
        'w_out': nrm(ks[14], (DEPTH, D, D)) * (D ** -0.5 * DEEPNORM_BETA),
        'ln1_g': 1.0 + 0.02 * nrm(ks[15], (DEPTH, D)),
        'ln1_b': 0.02 * nrm(ks[16], (DEPTH, D)),
        'w_route_group': nrm(ks[17], (DEPTH, D, N_GROUPS)) * D ** -0.5,
        'b_route_group': 0.01 * nrm(ks[18], (DEPTH, N_GROUPS)),
        'w_route_expert': nrm(ks[19], (DEPTH, D, N_EXPERTS)) * D ** -0.5,
        'b_route_expert': 0.01 * nrm(ks[20], (DEPTH, N_EXPERTS)),
        'w_expert_in': nrm(ks[21], (DEPTH, N_EXPERTS, D, 2 * D_EXPERT)) * D ** -0.5,
        'w_expert_out': nrm(ks[22], (DEPTH, N_EXPERTS, D_EXPERT, D)) * (D_EXPERT ** -0.5 * DEEPNORM_BETA),
        'ln2_g': 1.0 + 0.02 * nrm(ks[23], (DEPTH, D)),
        'ln2_b': 0.02 * nrm(ks[24], (DEPTH, D)),
    }


def reference(x, c, ctx, c_ctx, w_ada, b_ada, w_in, conv_w, attn_sink, mlstm_gate_b, mlstm_norm_w,
              w_proj_a, w_proj_b, w_proj_c, w_out, ln1_g, ln1_b, w_route_group, b_route_group,
              w_route_expert, b_route_expert, w_expert_in, w_expert_out, ln2_g, ln2_b):
    B, L, D = x.shape
    n_ctx = ctx.shape[1]
    rope = _rope_tables(L)
    s_lat = jax.nn.silu(c)
    s_ctx = jax.nn.silu(c_ctx)
    for i in range(DEPTH):
        need_ctx = i < DEPTH - 1
        mod_l = jnp.split((s_lat @ w_ada[i] + b_ada[i])[:, None, :], 6, axis=-1)
        mod_c = jnp.split(s_ctx @ w_ada[i] + b_ada[i], 6, axis=-1)
        hl = _modulate(_layer_norm(x), mod_l[0], mod_l[1])
        hc = _modulate(_layer_norm(ctx), mod_c[0], mod_c[1])
        yl, yc = _token_mixer(hl, hc, w_in[i], conv_w[i], attn_sink[i], mlstm_gate_b[i], mlstm_norm_w[i],
                              w_proj_a[i], w_proj_b[i], w_proj_c[i], w_out[i], rope, need_ctx)
        x = _layer_norm(DEEPNORM_ALPHA * x + mod_l[2] * yl, ln1_g[i], ln1_b[i])
        hl2 = _modulate(_layer_norm(x), mod_l[3], mod_l[4]).reshape(B * L, D)
        if need_ctx:
            ctx = _layer_norm(DEEPNORM_ALPHA * ctx + mod_c[2] * yc, ln1_g[i], ln1_b[i])
            hc2 = _modulate(_layer_norm(ctx), mod_c[3], mod_c[4]).reshape(B * n_ctx, D)
            f = _hier_moe(jnp.concatenate([hl2, hc2], axis=0), w_route_group[i], b_route_group[i],
                          w_route_expert[i], b_route_expert[i], w_expert_in[i], w_expert_out[i])
            fl = f[:B * L].reshape(B, L, D)
            fc = f[B * L:].reshape(B, n_ctx, D)
            ctx = _layer_norm(DEEPNORM_ALPHA * ctx + mod_c[5] * fc, ln2_g[i], ln2_b[i])
        else:
            fl = _hier_moe(hl2, w_route_group[i], b_route_group[i], w_route_expert[i],
                           b_route_expert[i], w_expert_in[i], w_expert_out[i]).reshape(B, L, D)
        x = _layer_norm(DEEPNORM_ALPHA * x + mod_l[5] * fl, ln2_g[i], ln2_b[i])
    return x
```

```python
import os
import numpy as np
from contextlib import ExitStack, contextmanager
import concourse.bass as bass
import concourse.mybir as mybir
from concourse.bass_utils import run_bass_kernel_spmd

F32 = mybir.dt.float32
BF16 = mybir.dt.bfloat16
AF = mybir.ActivationFunctionType
ALU = mybir.AluOpType
AX = mybir.AxisListType

D = 1024
NCT = 2
DEPTH = 2
ALPHA = float((2 * DEPTH) ** 0.25)
LN_EPS = 1e-6
NEXP = 32


class KB:
    def __init__(self, nc, es):
        self.nc = nc
        self.es = es
        self.eng = {'pe': nc.tensor, 'act': nc.scalar, 'dve': nc.vector, 'pool': nc.gpsimd, 'sp': nc.sync}
        self.sem = {n: es.enter_context(nc.semaphore('s_' + n)) for n in self.eng}
        self.cnt = {n: 0 for n in self.eng}
        self.waited = {n: {} for n in self.eng}
        self.lastw = {}
        self.readers = {}
        self.chans = {}
        self.chan_by_sid = {}
        self.nsem = 0
        self.mute = False

    def _wait(self, e, tok):
        sid, sem, val, src = tok
        if src == e and e == 'pe':
            return
        if src == 'dma':
            val = self.chan_by_sid[sid][1]
        w = self.waited[e]
        if w.get(sid, 0) >= val:
            return
        w[sid] = val
        self.eng[e].wait_ge(sem, val)

    def _deps(self, e, r, w):
        toks = []
        for k in r:
            if k in self.lastw:
                toks.append(self.lastw[k])
        for k in w:
            if k in self.lastw:
                toks.append(self.lastw[k])
            toks.extend(self.readers.get(k, {}).values())
        for t in toks:
            self._wait(e, t)

    def _commit(self, tok, r, w):
        for k in r:
            self.readers.setdefault(k, {})[tok[0]] = tok
        for k in w:
            self.lastw[k] = tok
            self.readers[k] = {}

    def op(self, e, fn, r=(), w=()):
        if self.mute:
            return None
        self._deps(e, r, w)
        ins = fn(self.eng[e])
        self.cnt[e] += 1
        ins.then_inc(self.sem[e], 1)
        tok = ('e_' + e, self.sem[e], self.cnt[e], e)
        self._commit(tok, r, w)
        return tok

    def mm(self, fns, r=(), w=(), drain=False):
        if self.mute:
            return None
        self._deps('pe', r, w)
        if drain and self.cnt['pe'] > 0 and self.waited['pe'].get('e_pe', 0) < self.cnt['pe']:
            self.waited['pe']['e_pe'] = self.cnt['pe']
            self.nc.tensor.wait_ge(self.sem['pe'], self.cnt['pe'])
        ins = None
        for fn in fns:
            ins = fn(self.nc.tensor)
        self.cnt['pe'] += 1
        ins.then_inc(self.sem['pe'], 1)
        tok = ('e_pe', self.sem['pe'], self.cnt['pe'], 'pe')
        self._commit(tok, r, w)
        return tok

    def dma(self, q, out, in_, chan, r=(), w=()):
        if self.mute:
            return None
        self._deps(q, r, w)
        if chan not in self.chans:
            self.chans[chan] = [self.es.enter_context(self.nc.semaphore('d%d' % len(self.chans))), 0]
        c = self.chans[chan]
        self.chan_by_sid['c_%s' % (chan,)] = c
        c[1] += 16
        self.eng[q].dma_start(out=out, in_=in_).then_inc(c[0], 16)
        tok = ('c_%s' % (chan,), c[0], c[1], 'dma')
        self._commit(tok, r, w)
        return tok

    def barrier(self):
        if self.mute:
            return
        toks = [('e_' + n, self.sem[n], self.cnt[n], n) for n in self.eng if self.cnt[n] > 0]
        toks += [('c_%s' % (ch,), c[0], c[1], 'dma') for ch, c in self.chans.items() if c[1] > 0]
        for e in self.eng:
            for t in toks:
                if t[3] == e:
                    continue
                w = self.waited[e]
                if w.get(t[0], 0) >= t[2]:
                    continue
                w[t[0]] = t[2]
                self.eng[e].wait_ge(t[1], t[2])

    def flush(self, e='sp'):
        for chan, c in self.chans.items():
            if c[1] > 0:
                self._wait(e, ('c_%s' % (chan,), c[0], c[1], 'dma'))


def _perm_cols():
    o_bg, o_cg, o_xin, o_q, o_k, o_v, o_mq, o_mk, o_mv, o_og, o_gt, o_mg = (
        0, 256, 512, 768, 1280, 1408, 1536, 1792, 2048, 2304, 2560, 2576)
    part = np.array([d + 16 if (d % 32) < 16 else d - 16 for d in range(64)])
    f = []
    f += list(range(o_bg, o_bg + 256)) + list(range(o_cg, o_cg + 256)) + list(range(o_xin, o_xin + 256))
    for c in range(4):
        f += [o_q + c * 64 + d for d in range(64)] + [o_q + (4 + c) * 64 + d for d in range(64)]
    for c in range(4):
        f += [o_q + c * 64 + part[d] for d in range(64)] + [o_q + (4 + c) * 64 + part[d] for d in range(64)]
    f += [o_k + d for d in range(128)]
    f += [o_k + h * 64 + part[d] for h in range(2) for d in range(64)]
    f += list(range(o_mq, o_mq + 256)) + list(range(o_mk, o_mk + 256))
    t = []
    t += list(range(o_v, o_v + 128)) + list(range(o_mk, o_mk + 256))
    t += list(range(o_mv, o_mv + 256)) + list(range(o_og, o_og + 256))
    t += list(range(o_mg, o_mg + 3072))
    t += list(range(o_gt, o_gt + 16))
    return np.array(f), np.array(t)


def _consts(NLT):
    L = NLT * 128
    nf = 16
    inv = 10000.0 ** (-np.arange(nf, dtype=np.float32) / nf)
    pos = np.arange(L)
    pr = (pos // 64).astype(np.float32)
    pc = (pos % 64).astype(np.float32)
    C = np.zeros((128, L), np.float32)
    S = np.zeros((128, L), np.float32)
    for p in range(128):
        d = p % 64
        posv = pr if d < 32 else pc
        fi = d % 16
        ang = posv * inv[fi]
        C[p] = np.cos(ang)
        S[p] = -np.sin(ang) if (d % 32) < 16 else np.sin(ang)
    i = np.arange(128)
    same = (i[:, None] // 64) == (i[None, :] // 64)
    le = i[:, None] <= i[None, :]
    ge = i[:, None] >= i[None, :]
    cm = np.zeros((128, 2, 128), np.float32)
    cm[:, 0, 0:64] = 1.0
    cm[:, 1, 64:128] = 1.0
    cmt = np.zeros((128, 2), np.float32)
    cmt[0:64, 0] = 1.0
    cmt[64:128, 1] = 1.0
    return {
        'ropeC': C, 'ropeS': S,
        'ident': np.eye(128, dtype=np.float32),
        'ones': np.ones((128, 128), np.float32),
        'tri0': (same & le).astype(np.float32),
        'tri1': (same & ge).astype(np.float32),
        'blk': same.astype(np.float32),
        'amprev': (i[None, :] <= i[:, None]).astype(np.float32),
        'amnext': (i[:, None] <= i[None, :]).astype(np.float32),
        'cm': cm, 'cmt': cmt,
    }


class _Stop(Exception):
    pass


def build(NLT, debug=False, stop=DEPTH, upto=None, nexp_decl=NEXP, only=None, cpt=None):
    NT = NCT + NLT
    T = NT * 128
    L = NLT * 128
    nc = bass.Bass("TRN2", target_bir_lowering=False)
    dbg_kind = "ExternalOutput" if debug else "Internal"

    BIG = ('w_ada', 'w_f', 'w_t', 'w_ei', 'w_eo', 'w_pa', 'w_pb', 'w_pc', 'w_o', 'xin')

    def din(name, shape, dt=F32):
        kind = "Internal" if (only is not None and name in BIG) else "ExternalInput"
        return nc.dram_tensor(name, list(shape), dt, kind=kind).ap()

    def dscr(name, shape, dt):
        return nc.dram_tensor(name, list(shape), dt, kind=dbg_kind).ap()

    xin = din("xin", [T, D])
    cs_d = din("cs", [128, 8, 2])
    w_ada = din("w_ada", [DEPTH, D, 6 * D])
    b_ada = din("b_ada", [DEPTH, 6 * D])
    w_f = din("w_f", [DEPTH, D, 2560])
    w_t = din("w_t", [DEPTH, D, 3984])
    conv_w = din("conv_w", [DEPTH, 128, 2, 3])
    sink = din("sink", [DEPTH, 8])
    gate_b = din("gate_b", [DEPTH, 16])
    norm_w = din("norm_w", [DEPTH, 256])
    w_pa = din("w_pa", [DEPTH, 256, D])
    w_pb = din("w_pb", [DEPTH, 512, D])
    w_pc = din("w_pc", [DEPTH, 256, D])
    w_o = din("w_o", [DEPTH, D, D])
    ln1_g = din("ln1_g", [DEPTH, D])
    ln1_b = din("ln1_b", [DEPTH, D])
    ln2_g = din("ln2_g", [DEPTH, D])
    ln2_b = din("ln2_b", [DEPTH, D])
    w_r = din("w_r", [DEPTH, D, 36])
    b_r = din("b_r", [DEPTH, 36])
    w_ei = din("w_ei", [DEPTH, nexp_decl, D, D])
    w_eo = din("w_eo", [DEPTH, nexp_decl, 512, D])
    cst = {k: din("c_" + k, v.shape) for k, v in _consts(NLT).items()}
    y_out = nc.dram_tensor("y", [L, D], F32, kind="ExternalOutput").ap()

    ZF = dscr("ZF", [20, 128, T], BF16)
    ZT = dscr("ZT", [T, 3968], BF16)
    X1 = dscr("X1", [T, D], F32)
    X2 = dscr("X2", [T, D], F32)
    H2T = dscr("H2T", [128, 8, T], BF16)
    HFD = dscr("HFD", [T, 256], F32)
    DBA = dscr("DBA", [128, 2, T], BF16) if debug else None
    DBB = dscr("DBB", [128, 4, T], BF16) if debug else None
    DBC = dscr("DBC", [128, 2, T], BF16) if debug else None

    with ExitStack() as es:
        kb = KB(nc, es)

        @contextmanager
        def scope():
            stopped = False
            with ExitStack() as s_:
                try:
                    yield s_
                except _Stop:
                    stopped = True
            if stopped:
                raise _Stop()
            kb.barrier()
        uid = [0]

        def sb(es_, shape, dt=F32, name=None):
            uid[0] += 1
            return es_.enter_context(nc.sbuf_tensor((name or "t") + "_%d" % uid[0], list(shape), dt))

        def ps(es_, shape, dt=F32, name=None):
            uid[0] += 1
            n = int(np.prod(shape[1:]))
            assert n <= 512 and dt == F32
            t = es_.enter_context(nc.psum_tensor((name or "p") + "_%d" % uid[0], [128, 512], F32))
            v = t[:, 0:n]
            if len(shape) == 3:
                v = v.rearrange("p (a b) -> p a b", a=shape[1])
            return v

        ident = sb(es, [128, 128], F32, "ident")
        ones = sb(es, [128, 128], F32, "ones")
        kb.dma('sp', ident[:], cst['ident'][:, :], 'cst', w=['ident'])
        kb.dma('sp', ones[:], cst['ones'][:, :], 'cst', w=['ones'])
        epsT = sb(es, [128, 1], F32, "epsT")
        kb.op('pool', lambda e: e.memset(epsT[:], LN_EPS), w=['epsT'])
        cs = sb(es, [128, 8, 2], F32, "cs")
        ss = sb(es, [128, 8, 2], F32, "ss")
        kb.dma('sp', cs[:], cs_d[:, :, :], 'cst', w=['cs'])
        kb.op('act', lambda e: e.activation(out=ss[:], in_=cs[:], func=AF.Silu), r=['cs'], w=['ss'])
        modcol = sb(es, [128, 2, 4, 8], F32, "modcol")
        modrow = sb(es, [128, 2, 2, D], F32, "modrow")
        gates = sb(es, [128, NT, 16], F32, "gates")
        logits = sb(es, [128, NT, 36], F32, "logits")
        wts = sb(es, [128, NT, 32], F32, "wts")

        order_ = ['M', 'P', 'A', 'B', 'C', 'D', 'E']

        def _chk(ph):
            if upto is not None and order_.index(ph) > order_.index(upto):
                raise _Stop()
            kb.mute = (only is not None and ph not in only)

        def _pt(n):
            if cpt is not None and n > cpt:
                raise _Stop()

        try:
          for l in range(DEPTH):
              if l >= stop:
                  break
              need_ctx = l < DEPTH - 1
              X_in = xin if l == 0 else X2
              X_out = X2

              def K(name):
                  return (name, l)

              _chk('M')
              with scope() as pes:
                  modrep = sb(pes, [128, 2, 6, D], F32, "modrep")
                  brep = sb(pes, [128, 6 * D], F32, "brep")
                  kb.dma('sp', brep[:], b_ada[l].partition_broadcast(128), K('brep'), w=[K('brep')])
                  for c in (1, 4):
                      kb.op('dve', lambda e, c=c: e.tensor_scalar_add(out=brep[:, c * D:(c + 1) * D], in0=brep[:, c * D:(c + 1) * D], scalar1=1.0),
                            r=[K('brep')], w=[K('brep')])
                  wa = [sb(pes, [128, 8, 512], F32, "wa") for _ in range(2)]
                  pm = [ps(pes, [128, 512], F32, "pm") for _ in range(2)]
                  pcol = ps(pes, [128, 64], F32, "pcol")
                  wsrc = w_ada[l].rearrange("(k p) n -> p k n", p=128)
                  for cg in range(12):
                      s = cg % 2
                      kb.dma('sp', wa[s][:], wsrc[:, :, cg * 512:(cg + 1) * 512], ('wa', s), w=[('wa', s)])
                      chunk, half = cg // 2, cg % 2
                      for j in range(2):
                          kb.mm([lambda e, k=k, j=j, s=s: e.matmul(pm[j][:, :], lhsT=ss[:, k, j:j + 1].to_broadcast([128, 128]), rhs=wa[s][:, k, :],
                                                                    start=(k == 0), stop=(k == 7)) for k in range(8)],
                                r=['ss', ('wa', s)], w=[('pm', j)])
                          kb.op('dve', lambda e, j=j, chunk=chunk, half=half, cg=cg: e.tensor_tensor(
                              out=modrep[:, j, chunk, half * 512:(half + 1) * 512], in0=pm[j][:, :], in1=brep[:, cg * 512:(cg + 1) * 512], op=ALU.add),
                              r=[('pm', j), K('brep')], w=[K('modrep')])
                  fns = []
                  for j in range(2):
                      for ci, c in enumerate((0, 1, 3, 4)):
                          for k in range(8):
                              idx = (j * 4 + ci) * 8 + k
                              fns.append(lambda e, j=j, c=c, k=k, idx=idx: e.matmul(
                                  pcol[:, idx:idx + 1], lhsT=modrep[:, j, c, k * 128:(k + 1) * 128], rhs=ident[:, 0:1], start=True, stop=True))
                  kb.mm(fns, r=[K('modrep'), 'ident'], w=['pcol'])
                  kb.op('dve', lambda e: e.tensor_copy(out=modcol[:].rearrange("p j c k -> p (j c k)"), in_=pcol[:, :]), r=['pcol'], w=['modcol'])
                  for j in range(2):
                      for gi, c in enumerate((2, 5)):
                          kb.op('dve', lambda e, j=j, gi=gi, c=c: e.tensor_copy(out=modrow[:, j, gi, :], in_=modrep[:, j, c, :]),
                                r=[K('modrep')], w=['modrow'])

              _chk('P')
              with scope() as pes:
                  hT = sb(pes, [128, 8, T], BF16, "hT")
                  with scope() as p1:
                      xt = [sb(p1, [128, D], F32, "xt") for _ in range(2)]
                      xn = [sb(p1, [128, D], F32, "xn") for _ in range(2)]
                      st = sb(p1, [128, 2, 6], F32, "st")
                      mv = sb(p1, [128, 2], F32, "mv")
                      rstd = sb(p1, [128, 1], F32, "rstd")
                      ptp = [ps(p1, [128, 4, 128], F32, "ptp") for _ in range(2)]
                      for tt in range(NT):
                          s = tt % 2
                          j = 1 if tt < NCT else 0
                          kb.dma('sp', xt[s][:], X_in[tt * 128:(tt + 1) * 128, :], ('xt', s), r=[K('X2w')] if l > 0 else [], w=[('xt', s)])
                          for h in range(2):
                              kb.op('dve', lambda e, h=h, s=s: e.bn_stats(out=st[:, h, :], in_=xt[s][:, h * 512:(h + 1) * 512]), r=[('xt', s)], w=['st'])
                          kb.op('dve', lambda e: e.bn_aggr(out=mv[:], in_=st[:].rearrange("p a b -> p (a b)")), r=['st'], w=['mv'])
                          kb.op('act', lambda e: e.activation(out=rstd[:], in_=mv[:, 1:2], func=AF.Sqrt, bias=epsT[:, 0:1]), r=['mv', 'epsT'], w=['rstd'])
                          kb.op('dve', lambda e: e.reciprocal(out=rstd[:], in_=rstd[:]), r=['rstd'], w=['rstd'])
                          kb.op('dve', lambda e, s=s: e.tensor_scalar(out=xn[s][:], in0=xt[s][:], scalar1=mv[:, 0:1], scalar2=rstd[:, 0:1], op0=ALU.subtract, op1=ALU.mult),
                                r=[('xt', s), 'mv', 'rstd'], w=[('xn', s)])
                          for hb in range(2):
                              kb.mm([lambda e, k=k, hb=hb, s=s: e.transpose(ptp[hb][:, k % 4, :], xn[s][:, k * 128:(k + 1) * 128], ident[:]) for k in range(hb * 4, hb * 4 + 4)],
                                    r=[('xn', s), 'ident'], w=[('ptp', hb)])
                              for k in range(hb * 4, hb * 4 + 4):
                                  kb.op('act', lambda e, k=k, hb=hb, j=j, tt=tt: e.activation(
                                      out=hT[:, k, tt * 128:(tt + 1) * 128], in_=ptp[hb][:, k % 4, :], func=AF.Identity,
                                      bias=modcol[:, j, 0, k:k + 1], scale=modcol[:, j, 1, k:k + 1]),
                                      r=[('ptp', hb), 'modcol'], w=[K('hT')])
                  with scope() as p2:
                      wf = [sb(p2, [128, 8, 512], BF16, "wf") for _ in range(2)]
                      zf = [sb(p2, [128, T], BF16, "zf") for _ in range(2)]
                      pz = [ps(p2, [128, 512], F32, "pz") for _ in range(4)]
                      wsrc = w_f[l].rearrange("(k p) n -> p k n", p=128)
                      tgroups = [(0, NCT * 128)] + [(NCT * 128 + g * 512, NCT * 128 + (g + 1) * 512) for g in range(L // 512)]
                      ev = 0
                      for jg in range(5):
                          s = jg % 2
                          kb.dma('pool', wf[s][:], wsrc[:, :, jg * 512:(jg + 1) * 512], ('wf', s), w=[('wf', s)])
                          for jj in range(4):
                              jch = jg * 4 + jj
                              zs = jch % 2
                              for (a, b) in tgroups:
                                  pb_ = ev % 4
                                  kb.mm([lambda e, k=k, s=s, jj=jj, a=a, b=b, pb_=pb_: e.matmul(pz[pb_][:, 0:b - a], lhsT=wf[s][:, k, jj * 128:(jj + 1) * 128], rhs=hT[:, k, a:b],
                                                                                          start=(k == 0), stop=(k == 7)) for k in range(8)],
                                        r=[('wf', s), K('hT')], w=[('pz', pb_)])
                                  eng = 'act' if ev % 2 == 0 else 'dve'
                                  if eng == 'act':
                                      kb.op('act', lambda e, zs=zs, a=a, b=b, pb_=pb_: e.copy(out=zf[zs][:, a:b], in_=pz[pb_][:, 0:b - a]), r=[('pz', pb_)], w=[('zf', zs)])
                                  else:
                                      kb.op('dve', lambda e, zs=zs, a=a, b=b, pb_=pb_: e.tensor_copy(out=zf[zs][:, a:b], in_=pz[pb_][:, 0:b - a]), r=[('pz', pb_)], w=[('zf', zs)])
                                  ev += 1
                              kb.dma('sp', ZF[jch], zf[zs][:], ('zfo', zs), r=[('zf', zs)], w=[K('ZF')])
                  with scope() as p3:
                      wt = [sb(p3, [128, 8, 512], BF16, "wt") for _ in range(2)]
                      zt = [sb(p3, [128, 512], BF16, "zt") for _ in range(4)]
                      pz = [ps(p3, [128, 512], F32, "pz") for _ in range(4)]
                      wsrc = w_t[l].rearrange("(k p) n -> p k n", p=128)
                      cgroups = [(0, 384), (384, 896)] + [(896 + g * 512, 896 + (g + 1) * 512) for g in range(6)] + [(3968, 3984)]
                      ev = 0
                      for gi, (ca, cb) in enumerate(cgroups):
                          s = gi % 2
                          n = cb - ca
                          kb.dma('pool', wt[s][:, :, 0:n], wsrc[:, :, ca:cb], ('wt', s), w=[('wt', s)])
                          for tt in range(NT):
                              pb_ = ev % 4
                              kb.mm([lambda e, k=k, s=s, n=n, tt=tt, pb_=pb_: e.matmul(pz[pb_][:, 0:n], lhsT=hT[:, k, tt * 128:(tt + 1) * 128], rhs=wt[s][:, k, 0:n],
                                                                                        start=(k == 0), stop=(k == 7)) for k in range(8)],
                                    r=[('wt', s), K('hT')], w=[('pz3', pb_)])
                              if gi == 8:
                                  kb.op('dve', lambda e, tt=tt, pb_=pb_: e.tensor_copy(out=gates[:, tt, :], in_=pz[pb_][:, 0:16]), r=[('pz3', pb_)], w=['gates'])
                              else:
                                  zs = ev % 4
                                  if ev % 2 == 0:
                                      kb.op('act', lambda e, zs=zs, n=n, pb_=pb_: e.copy(out=zt[zs][:, 0:n], in_=pz[pb_][:, 0:n]), r=[('pz3', pb_)], w=[('zt', zs)])
                                  else:
                                      kb.op('dve', lambda e, zs=zs, n=n, pb_=pb_: e.tensor_copy(out=zt[zs][:, 0:n], in_=pz[pb_][:, 0:n]), r=[('pz3', pb_)], w=[('zt', zs)])
                                  kb.dma('sp', ZT[tt * 128:(tt + 1) * 128, ca:cb], zt[zs][:, 0:n], ('zto', zs), r=[('zt', zs)], w=[K('ZT')])
                              ev += 1

              with scope() as mes:
                  yaT = sb(mes, [128, 2, T], BF16, "yaT")
                  ybT = sb(mes, [128, 4, T], BF16, "ybT")
                  ycT = sb(mes, [128, 2, T], BF16, "ycT")

                  _chk('A')
                  with scope() as pes:
                      bg = sb(pes, [128, T], BF16, "bg")
                      cg_ = sb(pes, [128, T], BF16, "cg")
                      xi = sb(pes, [128, T], BF16, "xi")
                      P = sb(pes, [128, T], F32, "P")
                      acc = sb(pes, [128, T], F32, "acc")
                      cw = sb(pes, [128, 2, 3], F32, "cw")
                      kb.dma('sp', cw[:], conv_w[l], K('cw'), w=[K('cw')])
                      for c in range(2):
                          kb.dma('sp', bg[:], ZF[0 + c], 'cvl', r=[K('ZF')], w=['bg'])
                          kb.dma('sp', cg_[:], ZF[2 + c], 'cvl', r=[K('ZF')], w=['cg'])
                          kb.dma('sp', xi[:], ZF[4 + c], 'cvl', r=[K('ZF')], w=['xi'])
                          kb.op('dve', lambda e: e.tensor_tensor(out=P[:], in0=cg_[:], in1=xi[:], op=ALU.mult), r=['cg', 'xi'], w=['P'])
                          for (a, b) in ((0, NCT * 128), (NCT * 128, T)):
                              kb.op('dve', lambda e, a=a, b=b, c=c: e.tensor_scalar(out=acc[:, a:b], in0=P[:, a:b], scalar1=cw[:, c, 1:2], scalar2=None, op0=ALU.mult),
                                    r=['P', K('cw')], w=['acc'])
                              kb.op('dve', lambda e, a=a, b=b, c=c: e.scalar_tensor_tensor(out=acc[:, a + 1:b], in0=P[:, a:b - 1], scalar=cw[:, c, 0:1], in1=acc[:, a + 1:b],
                                                                                          op0=ALU.mult, op1=ALU.add), r=['P', K('cw')], w=['acc'])
                              kb.op('dve', lambda e, a=a, b=b, c=c: e.scalar_tensor_tensor(out=acc[:, a:b - 1], in0=P[:, a + 1:b], scalar=cw[:, c, 2:3], in1=acc[:, a:b - 1],
                                                                                          op0=ALU.mult, op1=ALU.add), r=['P', K('cw')], w=['acc'])
                          kb.op('dve', lambda e, c=c: e.tensor_tensor(out=yaT[:, c, :], in0=bg[:], in1=acc[:], op=ALU.mult), r=['bg', 'acc'], w=[K('yaT')])

                  _chk('B')
                  with scope() as pes:
                      qT = sb(pes, [128, 4, T], BF16, "qT")
                      kT = sb(pes, [128, T], BF16, "kT")
                      vext = sb(pes, [128, NT, 2, 65], BF16, "vext")
                      esink = sb(pes, [128, 8], F32, "esink")
                      mprev = sb(pes, [128, 128], BF16, "mprev")
                      mnext = sb(pes, [128, 128], BF16, "mnext")
                      kb.dma('pool', mprev[:], cst['amprev'][:, :], K('am'), w=['mprev'])
                      kb.dma('pool', mnext[:], cst['amnext'][:, :], K('am'), w=['mnext'])
                      kb.dma('sp', esink[:], sink[l].partition_broadcast(128), K('sink'), w=['esink'])
                      kb.op('act', lambda e: e.activation(out=esink[:], in_=esink[:], func=AF.Exp), r=['esink'], w=['esink'])
                      with scope() as r1:
                          RP = min(L, 1024)
                          rc = sb(r1, [128, RP], F32, "rc")
                          rs = sb(r1, [128, RP], F32, "rs")
                          qa = sb(r1, [128, T], BF16, "qa")
                          qb = sb(r1, [128, T], BF16, "qb")
                          t1 = sb(r1, [128, RP], F32, "t1")
                          t2 = sb(r1, [128, RP], F32, "t2")
                          vtmp = sb(r1, [128, NT, 128], BF16, "vtmp")
                          c0 = NCT * 128
                          for ci in range(5):
                              ja, jb = (6 + ci, 10 + ci) if ci < 4 else (14, 15)
                              kb.dma('sp', qa[:], ZF[ja], 'rpl', r=[K('ZF')], w=['qa'])
                              kb.dma('sp', qb[:], ZF[jb], 'rpl', r=[K('ZF')], w=['qb'])
                              dst = qT[:, ci, :] if ci < 4 else kT[:, :]
                              for pa in range(0, L, RP):
                                  kb.dma('sp', rc[:], cst['ropeC'][:, pa:pa + RP], K('rope'), w=['rc'])
                                  kb.dma('sp', rs[:], cst['ropeS'][:, pa:pa + RP], K('rope'), w=['rs'])
                                  kb.op('dve', lambda e: e.tensor_tensor(out=t1[:], in0=qa[:, c0 + pa:c0 + pa + RP], in1=rc[:], op=ALU.mult), r=['qa', 'rc'], w=['t1'])
                                  kb.op('dve', lambda e: e.tensor_tensor(out=t2[:], in0=qb[:, c0 + pa:c0 + pa + RP], in1=rs[:], op=ALU.mult), r=['qb', 'rs'], w=['t2'])
                                  kb.op('dve', lambda e, dst=dst: e.tensor_tensor(out=dst[:, c0 + pa:c0 + pa + RP], in0=t1[:], in1=t2[:], op=ALU.add), r=['t1', 't2'], w=[K('qkT')])
                              kb.op('dve', lambda e, dst=dst: e.tensor_copy(out=dst[:, 0:c0], in_=qa[:, 0:c0]), r=['qa'], w=[K('qkT')])
                          kb.dma('sp', vtmp[:], ZT[:, 0:128].rearrange("(n p) c -> p n c", p=128), 'rpl', r=[K('ZT')], w=['vtmp'])
                          kb.op('dve', lambda e: e.memset(vext[:].rearrange("p n g d -> p (n g d)"), 1.0), w=[K('vext')])
                          kb.op('dve', lambda e: e.tensor_copy(out=vext[:, :, :, 0:64], in_=vtmp[:].rearrange("p n (g d) -> p n g d", g=2)), r=['vtmp'], w=[K('vext')])
                      with scope() as r2:
                          pS = [ps(r2, [128, 4, 128], F32, "pS") for _ in range(3)]
                          ppv4 = [ps(r2, [128, 4, 65], F32, "ppv") for _ in range(4)]
                          ptr = ps(r2, [128, 4, 128], F32, "ptr")
                          eb = [sb(r2, [128, 4, 128], BF16, "eb") for _ in range(3)]
                          den = sb(r2, [128, 8], F32, "den")
                          yb = sb(r2, [128, 8, 64], F32, "yb")
                          qtiles = list(range(NT)) if need_ctx else list(range(NCT, NT))
                          it = 0
                          for qt in qtiles:
                              if qt < NCT:
                                  ktl = [(0, None), (1, None)]
                              else:
                                  ktl = [(0, None), (1, None)]
                                  if qt - 1 >= NCT:
                                      ktl.append((qt - 1, mprev))
                                  ktl.append((qt, None))
                                  if qt + 1 < NT:
                                      ktl.append((qt + 1, mnext))
                              qs = slice(qt * 128, (qt + 1) * 128)
                              qpar = qt % 2
                              ppv = ppv4[qpar * 2:qpar * 2 + 2]
                              for g in range(2):
                                  pr = slice(g * 64, (g + 1) * 64)
                                  for idx, (kt, msk) in enumerate(ktl):
                                      b3 = it % 3
                                      it += 1
                                      kb.mm([lambda e, b3=b3, pr=pr, kt=kt, qs=qs: e.matmul(pS[b3][:], lhsT=kT[pr, kt * 128:(kt + 1) * 128], rhs=qT[pr, :, qs], start=True, stop=True)],
                                            r=[K('qkT')], w=[('pS', b3)])
                                      kb.op('act', lambda e, b3=b3: e.activation(out=eb[b3][:], in_=pS[b3][:], func=AF.Exp, scale=0.125), r=[('pS', b3)], w=[('eb', b3)])
                                      if msk is not None:
                                          kb.op('pool', lambda e, b3=b3, msk=msk: e.tensor_tensor(out=eb[b3][:], in0=eb[b3][:], in1=msk[:, :].unsqueeze(1).to_broadcast([128, 4, 128]), op=ALU.mult),
                                                r=[('eb', b3), 'mprev', 'mnext'], w=[('eb', b3)])
                                      kb.mm([lambda e, b3=b3, c=c, g=g, kt=kt, idx=idx, n=len(ktl): e.matmul(ppv[g][:, c, :], lhsT=eb[b3][:, c, :], rhs=vext[:, kt, g, :],
                                                                                                         start=(idx == 0 and c == 0), stop=(idx == n - 1), skip_group_check=True) for c in range(4)],
                                            r=[('eb', b3), K('vext')], w=[('ppv', qpar, g)])
                              for g in range(2):
                                  kb.op('dve', lambda e, g=g: e.tensor_tensor(out=den[:, g * 4:(g + 1) * 4], in0=ppv[g][:, :, 64], in1=esink[:, g * 4:(g + 1) * 4], op=ALU.add),
                                        r=[('ppv', qpar, g), 'esink'], w=['den'])
                              kb.op('dve', lambda e: e.reciprocal(out=den[:], in_=den[:]), r=['den'], w=['den'])
                              for g in range(2):
                                  kb.op('dve', lambda e, g=g: e.tensor_tensor(out=yb[:, g * 4:(g + 1) * 4, :], in0=ppv[g][:, :, 0:64],
                                                                                in1=den[:, g * 4:(g + 1) * 4].unsqueeze(2).to_broadcast([128, 4, 64]), op=ALU.mult),
                                        r=[('ppv', qpar, g), 'den'], w=['yb'])
                              ybf = yb[:].rearrange("p h d -> p (h d)")
                              kb.mm([lambda e, c=c: e.transpose(ptr[:, c, :], ybf[:, c * 128:(c + 1) * 128], ident[:]) for c in range(4)], r=['yb', 'ident'], w=['ptr'])
                              kb.op('act', lambda e, qs=qs: e.copy(out=ybT[:, :, qs], in_=ptr[:]), r=['ptr'], w=[K('ybT')])

                  _chk('C')
                  with scope() as pes:
                      mqT = sb(pes, [128, 2, T], BF16, "mqT")
                      mkT = sb(pes, [128, 2, T], BF16, "mkT")
                      mkt = sb(pes, [128, NT, 256], BF16, "mkt")
                      vx = sb(pes, [128, NT, 4, 65], BF16, "vx")
                      LF = sb(pes, [128, NT, 2, 4], F32, "LF")
                      LI = sb(pes, [128, NT, 2, 4], F32, "LI")
                      tri = [sb(pes, [128, 128], F32, "tri") for _ in range(2)]
                      mskd = [sb(pes, [128, 128], F32, "mskd") for _ in range(2)]
                      blk = sb(pes, [128, 128], F32, "blk")
                      cm = sb(pes, [128, 2, 128], F32, "cm")
                      cmt = sb(pes, [128, 2], F32, "cmt")
                      gbr = sb(pes, [128, 16], F32, "gbr")
                      nwr = sb(pes, [128, 256], F32, "nwr")
                      for d_ in range(2):
                          kb.dma('sp', tri[d_][:], cst['tri%d' % d_][:, :], K('mc'), w=[('tri', d_)])
                          kb.op('pool', lambda e, d_=d_: e.tensor_scalar(out=mskd[d_][:], in0=tri[d_][:], scalar1=0.125, scalar2=None, op0=ALU.mult), r=[('tri', d_)], w=[('mskd', d_)])
                      kb.dma('sp', blk[:], cst['blk'][:, :], K('mc'), w=['blk'])
                      kb.dma('sp', cm[:], cst['cm'][:, :, :], K('mc'), w=['cm'])
                      kb.dma('sp', cmt[:], cst['cmt'][:, :], K('mc'), w=['cmt'])
                      kb.dma('sp', gbr[:], gate_b[l].partition_broadcast(128), K('mc'), w=['gbr'])
                      kb.dma('sp', nwr[:], norm_w[l].partition_broadcast(128), K('mc'), w=['nwr'])
                      for c in range(2):
                          kb.dma('sp', mqT[:, c, :], ZF[16 + c], K('mcl'), r=[K('ZF')], w=[K('mqT')])
                          kb.dma('sp', mkT[:, c, :], ZF[18 + c], K('mcl'), r=[K('ZF')], w=[K('mkT')])
                      kb.dma('sp', mkt[:], ZT[:, 128:384].rearrange("(n p) c -> p n c", p=128), K('mcl'), r=[K('ZT')], w=[K('mkt')])
                      _pt(0)
                      with scope() as r1:
                          vtmp = sb(r1, [128, NT, 256], BF16, "vtmp2")
                          ga = sb(r1, [128, NT, 16], F32, "ga")
                          ex = sb(r1, [128, NT, 2, 4], F32, "ex")
                          kb.dma('sp', vtmp[:], ZT[:, 384:640].rearrange("(n p) c -> p n c", p=128), K('mcl'), r=[K('ZT')], w=['vtmp2'])
                          kb.op('pool', lambda e: e.memset(vx[:].rearrange("p n h d -> p (n h d)"), 1.0), w=[K('vx')])
                          kb.op('pool', lambda e: e.tensor_copy(out=vx[:, :, :, 0:64], in_=vtmp[:].rearrange("p n (h d) -> p n h d", h=4)), r=['vtmp2'], w=[K('vx')])
                          kb.op('dve', lambda e: e.tensor_tensor(out=ga[:], in0=gates[:], in1=gbr[:, :].unsqueeze(1).to_broadcast([128, NT, 16]), op=ALU.add), r=['gates', 'gbr'], w=['ga'])
                          gav = ga[:].rearrange("p n (d t h) -> p n d t h", d=2, t=2)
                          kb.op('dve', lambda e: e.tensor_copy(out=LI[:], in_=gav[:, :, :, 0, :]), r=['ga'], w=[K('LI')])
                          kb.op('act', lambda e: e.activation(out=ex[:], in_=gav[:, :, :, 1, :], func=AF.Exp, scale=-1.0), r=['ga'], w=['ex'])
                          kb.op('act', lambda e: e.activation(out=ex[:], in_=ex[:], func=AF.Ln, bias=ones[:, 0:1]), r=['ex', 'ones'], w=['ex'])
                          kb.op('dve', lambda e: e.tensor_scalar(out=LF[:], in0=ex[:], scalar1=-1.0, scalar2=None, op0=ALU.mult), r=['ex'], w=[K('LF')])
                      _pt(1)
                      with scope() as r2:
                          psm = ps(r2, [128, 16], F32, "psm")
                          psg = ps(r2, [128, 16], F32, "psg")
                          pbr = ps(r2, [128, 4, 128], F32, "pbr")
                          pstb = [ps(r2, [128, 2, 128], F32, "pst") for _ in range(2)]
                          pkv = ps(r2, [128, 2, 130], F32, "pkv")
                          pnum = ps(r2, [128, 4, 65], F32, "pnum")
                          ptr = ps(r2, [128, 2, 128], F32, "ptrc")
                          Cst = sb(r2, [128, 2, 65], F32, "Cst")
                          CB = [sb(r2, [128, 2, 65], BF16, "CB") for _ in range(2)]
                          QZ = sb(r2, [128, 2, 2, 128], BF16, "QZ")
                          biasr = sb(r2, [128, 4], F32, "biasr")
                          utmp = sb(r2, [128, 4], F32, "utmp")
                          U = sb(r2, [128, 4], F32, "U")
                          lfm = sb(r2, [128, 2, 4], F32, "lfm")
                          EG = sb(r2, [128, 2, 4], F32, "EG")
                          DT = sb(r2, [128, 4, 128], F32, "DT")
                          EB = sb(r2, [128, 4, 128], F32, "EB")
                          EBM = sb(r2, [128, 4, 2, 128], F32, "EBM")
                          STM = sb(r2, [128, 4, 128], BF16, "STM")
                          Kp = sb(r2, [128, 4, 64], BF16, "Kp")
                          dn = sb(r2, [128, 4], F32, "dn")
                          hd = sb(r2, [128, 4, 64], F32, "hd")
                          mu = sb(r2, [128, 4], F32, "mu")
                          cen = sb(r2, [128, 4, 64], F32, "cen")
                          sq = sb(r2, [128, 4, 64], F32, "sq")
                          var = sb(r2, [128, 4], F32, "var")
                          ogt = sb(r2, [128, 256], BF16, "ogt")
                          hfl = sb(r2, [128, 256], F32, "hfl")
                          ogs = sb(r2, [128, 256], F32, "ogs")
                          yc = sb(r2, [128, 256], F32, "yc")
                          for d_ in range(2):
                              order = list(range(NT)) if d_ == 0 else (list(range(NCT - 1, -1, -1)) + list(range(NT - 1, NCT - 1, -1)))
                              chorder = (0, 1) if d_ == 0 else (1, 0)
                              kb.op('dve', lambda e: e.memset(Cst[:].rearrange("p c d -> p (c d)"), 0.0), w=['Cst'])
                              for tt in order:
                                  tsl = slice(tt * 128, (tt + 1) * 128)
                                  need_out = need_ctx or tt >= NCT
                                  lf_t = LF[:, tt, d_, :]
                                  kb.mm([lambda e: e.matmul(psm[:, 0:4], lhsT=tri[d_][:], rhs=lf_t, start=True, stop=True),
                                         lambda e: e.matmul(psm[:, 4:8], lhsT=blk[:], rhs=lf_t, start=True, stop=True)],
                                        r=[K('LF'), ('tri', d_), 'blk'], w=['psm'])
                                  kb.op('dve', lambda e: e.tensor_tensor(out=biasr[:], in0=LI[:, tt, d_, :], in1=psm[:, 0:4], op=ALU.subtract), r=[K('LI'), 'psm'], w=['biasr'])
                                  kb.op('dve', lambda e: e.tensor_tensor(out=utmp[:], in0=biasr[:], in1=psm[:, 4:8], op=ALU.add), r=['biasr', 'psm'], w=['utmp'])
                                  kb.op('act', lambda e: e.activation(out=U[:], in_=utmp[:], func=AF.Exp), r=['utmp'], w=['U'])
                                  _pt(2)
                                  kb.op('dve', lambda e: e.tensor_tensor(out=lfm[:], in0=lf_t.unsqueeze(1).to_broadcast([128, 2, 4]), in1=cmt[:, :].unsqueeze(2).to_broadcast([128, 2, 4]), op=ALU.mult),
                                        r=[K('LF'), 'cmt'], w=['lfm'])
                                  kb.mm([lambda e: e.matmul(psg[:, 0:8], lhsT=ones[:], rhs=lfm[:].rearrange("p a b -> p (a b)"), start=True, stop=True)], r=['lfm', 'ones'], w=['psm2'])
                                  kb.op('act', lambda e: e.activation(out=EG[:].rearrange("p a b -> p (a b)"), in_=psg[:, 0:8], func=AF.Exp), r=['psm2'], w=['EG'])
                                  _pt(3)
                                  if not os.environ.get('SKIP_PBR'):
                                      kb.mm([lambda e, h=h: e.matmul(pbr[:, h, :], lhsT=LF[:, tt, d_, h:h + 1].to_broadcast([128, 128]), rhs=tri[d_][:], start=True, stop=True) for h in range(4)],
                                            r=[K('LF'), ('tri', d_)], w=['pbr'])
                                  if not os.environ.get('SKIP_PST'):
                                    kb.mm([lambda e, h=h: e.matmul(pstb[h % 2][:, h // 2, :], lhsT=mkT[(h % 2) * 64:(h % 2) * 64 + 64, h // 2, tsl], rhs=mqT[(h % 2) * 64:(h % 2) * 64 + 64, h // 2, tsl], start=True, stop=True)
                                         for h in range(4)], r=[K('mkT'), K('mqT')], w=['pst'])
                                  for h in range(4):
                                      kb.op('act', lambda e, h=h: e.activation(out=DT[:, h, :], in_=pbr[:, h, :], func=AF.Exp, bias=biasr[:, h:h + 1]), r=['pbr', 'biasr'], w=['DT'])
                                  _pt(4)
                                  kb.op('act', lambda e: e.activation(out=EB[:], in_=pbr[:], func=AF.Exp), r=['pbr'], w=['EB'])
                                  kb.op('pool', lambda e: e.tensor_tensor(out=DT[:], in0=DT[:], in1=mskd[d_][:, :].unsqueeze(1).to_broadcast([128, 4, 128]), op=ALU.mult), r=['DT', ('mskd', d_)], w=['DT'])
                                  _pt(5)
                                  for half in range(2):
                                      kb.op('dve', lambda e, half=half: e.tensor_tensor(out=STM[:].rearrange("p (c x) d -> p c x d", x=2)[:, :, half, :], in0=pstb[half][:],
                                                                                            in1=DT[:].rearrange("p (c x) d -> p c x d", x=2)[:, :, half, :], op=ALU.mult), r=['pst', 'DT'], w=['STM'])
                                  kb.op('pool', lambda e: e.tensor_tensor(out=EBM[:], in0=EB[:].unsqueeze(2).to_broadcast([128, 4, 2, 128]), in1=cm[:].unsqueeze(1).to_broadcast([128, 4, 2, 128]), op=ALU.mult),
                                        r=['EB', 'cm'], w=['EBM'])
                                  for h in range(4):
                                      hr = slice((h % 2) * 64, (h % 2) * 64 + 64)
                                      kb.op('pool', lambda e, h=h, hr=hr: e.tensor_tensor(out=QZ[hr, h // 2, :, :], in0=mqT[hr, h // 2, tsl].unsqueeze(1).to_broadcast([64, 2, 128]), in1=EBM[hr, h, :, :], op=ALU.mult),
                                            r=[K('mqT'), 'EBM'], w=['QZ'])
                                  kb.op('dve', lambda e: e.scalar_tensor_tensor(out=Kp[:], in0=mkt[:, tt, :].rearrange("p (h d) -> p h d", h=4), scalar=0.125, in1=U[:, :].unsqueeze(2).to_broadcast([128, 4, 64]), op0=ALU.mult, op1=ALU.mult),
                                        r=[K('mkt'), 'U'], w=['Kp'])
                                  _pt(7)
                                  Kpf = Kp[:].rearrange("p h d -> p (h d)")
                                  for ci, ch in enumerate(chorder):
                                      kb.op('act', lambda e, ci=ci: e.copy(out=CB[ci][:], in_=Cst[:]), r=['Cst'], w=[('CB', ci)])
                                      cr = slice(ch * 64, ch * 64 + 64)
                                      kb.mm([lambda e, c=c, cr=cr: e.matmul(pkv[:, c, :], lhsT=Kpf[cr, c * 128:(c + 1) * 128], rhs=vx[cr, tt, 2 * c:2 * c + 2, :], start=True, stop=True) for c in range(2)],
                                            r=['Kp', K('vx')], w=['pkv'])
                                      for half in range(2):
                                          hr = slice(half * 64, half * 64 + 64)
                                          egb = EG[hr, ch, :].rearrange("p (c x) -> p c x", x=2)[:, :, half]
                                          kb.op('dve', lambda e, hr=hr, egb=egb: e.tensor_tensor(out=Cst[hr, :, :], in0=Cst[hr, :, :], in1=egb.unsqueeze(2).to_broadcast([64, 2, 65]), op=ALU.mult),
                                                r=['Cst', 'EG'], w=['Cst'])
                                          kb.op('dve', lambda e, hr=hr, half=half: e.tensor_tensor(out=Cst[hr, :, :], in0=Cst[hr, :, :], in1=pkv[hr, :, half * 65:(half + 1) * 65], op=ALU.add),
                                                r=['Cst', 'pkv'], w=['Cst'])
                                  _pt(8)
                                  if not need_out:
                                      continue
                                  fns = []
                                  for h in range(4):
                                      hr = slice((h % 2) * 64, (h % 2) * 64 + 64)
                                      fns.append(lambda e, h=h: e.matmul(pnum[:, h, :], lhsT=STM[:, h, :], rhs=vx[:, tt, h, :], start=True, stop=False))
                                      for ci, ch in enumerate(chorder):
                                          fns.append(lambda e, h=h, hr=hr, ci=ci, ch=ch: e.matmul(pnum[:, h, :], lhsT=QZ[hr, h // 2, ch, :], rhs=CB[ci][hr, h // 2, :], start=False, stop=(ci == 1)))
                                  kb.mm(fns, r=['STM', K('vx'), 'QZ', ('CB', 0), ('CB', 1)], w=['pnum'])
                                  _pt(9)
                                  kb.op('act', lambda e: e.activation(out=dn[:], in_=pnum[:, :, 64], func=AF.Abs), r=['pnum'], w=['dn'])
                                  kb.op('dve', lambda e: e.tensor_scalar(out=dn[:], in0=dn[:], scalar1=1.0, scalar2=None, op0=ALU.max), r=['dn'], w=['dn'])
                                  kb.op('dve', lambda e: e.reciprocal(out=dn[:], in_=dn[:]), r=['dn'], w=['dn'])
                                  if d_ == 0:
                                      kb.op('dve', lambda e: e.tensor_tensor(out=hd[:], in0=pnum[:, :, 0:64], in1=dn[:, :].unsqueeze(2).to_broadcast([128, 4, 64]), op=ALU.mult),
                                            r=['pnum', 'dn'], w=['hd'])
                                      kb.dma('sp', HFD[tsl, :], hd[:].rearrange("p h d -> p (h d)"), 'hfo', r=['hd'], w=[K('HFD')])
                                      continue
                                  kb.dma('sp', ogt[:], ZT[tsl, 640:896], 'ogl', r=[K('ZT')], w=['ogt'])
                                  kb.dma('sp', hfl[:], HFD[tsl, :], 'hfl', r=[K('HFD')], w=['hfl'])
                                  kb.op('dve', lambda e: e.tensor_tensor(out=hd[:], in0=pnum[:, :, 0:64], in1=dn[:, :].unsqueeze(2).to_broadcast([128, 4, 64]), op=ALU.mult), r=['pnum', 'dn'], w=['hd'])
                                  kb.op('dve', lambda e: e.tensor_tensor(out=hd[:], in0=hd[:], in1=hfl[:].rearrange("p (h d) -> p h d", h=4), op=ALU.add), r=['hd', 'hfl'], w=['hd'])
                                  kb.op('dve', lambda e: e.tensor_reduce(out=mu[:], in_=hd[:], axis=AX.X, op=ALU.add), r=['hd'], w=['mu'])
                                  kb.op('dve', lambda e: e.tensor_scalar(out=mu[:], in0=mu[:], scalar1=1.0 / 64, scalar2=None, op0=ALU.mult), r=['mu'], w=['mu'])
                                  kb.op('dve', lambda e: e.tensor_tensor(out=cen[:], in0=hd[:], in1=mu[:, :].unsqueeze(2).to_broadcast([128, 4, 64]), op=ALU.subtract), r=['hd', 'mu'], w=['cen'])
                                  kb.op('pool', lambda e: e.tensor_tensor(out=sq[:], in0=cen[:], in1=cen[:], op=ALU.mult), r=['cen'], w=['sq'])
                                  kb.op('dve', lambda e: e.tensor_reduce(out=var[:], in_=sq[:], axis=AX.X, op=ALU.add), r=['sq'], w=['var'])
                                  kb.op('act', lambda e: e.activation(out=var[:], in_=var[:], func=AF.Sqrt, bias=epsT[:, 0:1], scale=1.0 / 64), r=['var', 'epsT'], w=['var'])
                                  kb.op('dve', lambda e: e.reciprocal(out=var[:], in_=var[:]), r=['var'], w=['var'])
                                  kb.op('dve', lambda e: e.tensor_tensor(out=cen[:], in0=cen[:], in1=var[:, :].unsqueeze(2).to_broadcast([128, 4, 64]), op=ALU.mult), r=['cen', 'var'], w=['cen'])
                                  kb.op('act', lambda e: e.activation(out=ogs[:], in_=ogt[:], func=AF.Sigmoid), r=['ogt'], w=['ogs'])
                                  kb.op('pool', lambda e: e.tensor_tensor(out=ogs[:], in0=ogs[:], in1=nwr[:], op=ALU.mult), r=['ogs', 'nwr'], w=['ogs'])
                                  kb.op('dve', lambda e: e.tensor_tensor(out=yc[:], in0=cen[:].rearrange("p h d -> p (h d)"), in1=ogs[:], op=ALU.mult), r=['cen', 'ogs'], w=['yc'])
                                  kb.mm([lambda e, c=c: e.transpose(ptr[:, c, :], yc[:, c * 128:(c + 1) * 128], ident[:]) for c in range(2)], r=['yc', 'ident'], w=['ptrc'])
                                  kb.op('act', lambda e: e.copy(out=ycT[:, :, tsl], in_=ptr[:]), r=['ptrc'], w=[K('ycT')])

                  if debug and l == 0:
                      kb.dma('sp', DBA[:, :, :], yaT[:], 'dbg', r=[K('yaT')], w=['DBA'])
                      kb.dma('sp', DBB[:, :, :], ybT[:], 'dbg', r=[K('ybT')], w=['DBB'])
                      kb.dma('sp', DBC[:, :, :], ycT[:], 'dbg', r=[K('ycT')], w=['DBC'])
                  _chk('D')
                  with scope() as pes:
                      wpa = sb(pes, [128, 2, D], BF16, "wpa")
                      wpb = sb(pes, [128, 4, D], BF16, "wpb")
                      wpc = sb(pes, [128, 2, D], BF16, "wpc")
                      wo = sb(pes, [128, 8, D], BF16, "wo")
                      wr = sb(pes, [128, 8, 36], F32, "wr")
                      brr = sb(pes, [128, 36], F32, "brr")
                      g1r = sb(pes, [128, D], F32, "g1r")
                      b1r = sb(pes, [128, D], F32, "b1r")
                      kb.dma('pool', wpa[:], w_pa[l].rearrange("(k p) n -> p k n", p=128), K('dw'), w=['wpa'])
                      kb.dma('pool', wpb[:], w_pb[l].rearrange("(k p) n -> p k n", p=128), K('dw'), w=['wpb'])
                      kb.dma('pool', wpc[:], w_pc[l].rearrange("(k p) n -> p k n", p=128), K('dw'), w=['wpc'])
                      kb.dma('pool', wo[:], w_o[l].rearrange("(k p) n -> p k n", p=128), K('dw'), w=['wo'])
                      kb.dma('sp', wr[:], w_r[l].rearrange("(k p) n -> p k n", p=128), K('dw2'), w=['wr'])
                      kb.dma('sp', brr[:], b_r[l].partition_broadcast(128), K('dw2'), w=['brr'])
                      kb.dma('sp', g1r[:], ln1_g[l].partition_broadcast(128), K('dw2'), w=['g1r'])
                      kb.dma('sp', b1r[:], ln1_b[l].partition_broadcast(128), K('dw2'), w=['b1r'])
                      mg = [sb(pes, [128, 3072], BF16, "mg") for _ in range(2)]
                      sg = sb(pes, [128, 3072], F32, "sg")
                      xr = [sb(pes, [128, D], F32, "xr") for _ in range(2)]
                      u = sb(pes, [128, D], F32, "u")
                      tmp = sb(pes, [128, 512], F32, "tmp")
                      uT = sb(pes, [128, 8, 128], BF16, "uT")
                      rr = sb(pes, [128, D], F32, "rr")
                      x1 = sb(pes, [128, D], F32, "x1")
                      xn2 = sb(pes, [128, D], F32, "xn2")
                      h2f = sb(pes, [128, 8, 128], F32, "h2f")
                      h2b = sb(pes, [128, 8, 128], BF16, "h2b")
                      st = sb(pes, [128, 2, 6], F32, "st")
                      mv = sb(pes, [128, 2], F32, "mv")
                      rstd = sb(pes, [128, 1], F32, "rstd")
                      pabc = [ps(pes, [128, 512], F32, "pabc") for _ in range(3)]
                      ptp = [ps(pes, [128, 4, 128], F32, "ptp") for _ in range(2)]
                      pyl = [ps(pes, [128, 512], F32, "pyl") for _ in range(2)]
                      prt = ps(pes, [128, 36], F32, "prt")

                      def layer_norm(src, srckey, dst, dstkey):
                          for h in range(2):
                              kb.op('dve', lambda e, h=h: e.bn_stats(out=st[:, h, :], in_=src[:, h * 512:(h + 1) * 512]), r=[srckey], w=['st'])
                          kb.op('dve', lambda e: e.bn_aggr(out=mv[:], in_=st[:].rearrange("p a b -> p (a b)")), r=['st'], w=['mv'])
                          kb.op('act', lambda e: e.activation(out=rstd[:], in_=mv[:, 1:2], func=AF.Sqrt, bias=epsT[:, 0:1]), r=['mv', 'epsT'], w=['rstd'])
                          kb.op('dve', lambda e: e.reciprocal(out=rstd[:], in_=rstd[:]), r=['rstd'], w=['rstd'])
                          kb.op('dve', lambda e: e.tensor_scalar(out=dst[:], in0=src[:], scalar1=mv[:, 0:1], scalar2=rstd[:, 0:1], op0=ALU.subtract, op1=ALU.mult),
                                r=[srckey, 'mv', 'rstd'], w=[dstkey])

                      for tt in range(NT if need_ctx else NT):
                          if (not need_ctx) and tt < NCT:
                              continue
                          s = tt % 2
                          j = 1 if tt < NCT else 0
                          tsl = slice(tt * 128, (tt + 1) * 128)
                          kb.dma('sp', mg[s][:], ZT[tsl, 896:3968], ('mg', s), r=[K('ZT')], w=[('mg', s)])
                          kb.dma('sp', xr[s][:], X_in[tsl, :], ('xr', s), r=[K('X2w')] if l > 0 else [], w=[('xr', s)])
                          kb.op('act', lambda e, s=s: e.activation(out=sg[:], in_=mg[s][:], func=AF.Sigmoid), r=[('mg', s)], w=['sg'])
                          for n in range(2):
                              ns = slice(n * 512, (n + 1) * 512)
                              kb.mm([lambda e, k=k, ns=ns: e.matmul(pabc[0][:, :], lhsT=yaT[:, k, tsl], rhs=wpa[:, k, ns], start=(k == 0), stop=(k == 1)) for k in range(2)], r=[K('yaT'), 'wpa'], w=[('pabc', 0)])
                              kb.mm([lambda e, k=k, ns=ns: e.matmul(pabc[1][:, :], lhsT=ybT[:, k, tsl], rhs=wpb[:, k, ns], start=(k == 0), stop=(k == 3)) for k in range(4)], r=[K('ybT'), 'wpb'], w=[('pabc', 1)])
                              kb.mm([lambda e, k=k, ns=ns: e.matmul(pabc[2][:, :], lhsT=ycT[:, k, tsl], rhs=wpc[:, k, ns], start=(k == 0), stop=(k == 1)) for k in range(2)], r=[K('ycT'), 'wpc'], w=[('pabc', 2)])
                              kb.op('dve', lambda e, ns=ns: e.tensor_tensor(out=u[:, ns], in0=pabc[0][:, :], in1=sg[:, n * 512:(n + 1) * 512], op=ALU.mult), r=[('pabc', 0), 'sg'], w=['u'])
                              kb.op('dve', lambda e, n=n: e.tensor_tensor(out=tmp[:], in0=pabc[1][:, :], in1=sg[:, D + n * 512:D + (n + 1) * 512], op=ALU.mult), r=[('pabc', 1), 'sg'], w=['tmp'])
                              kb.op('pool', lambda e, ns=ns: e.tensor_tensor(out=u[:, ns], in0=u[:, ns], in1=tmp[:], op=ALU.add), r=['u', 'tmp'], w=['u'])
                              kb.op('dve', lambda e, n=n: e.tensor_tensor(out=tmp[:], in0=pabc[2][:, :], in1=sg[:, 2 * D + n * 512:2 * D + (n + 1) * 512], op=ALU.mult), r=[('pabc', 2), 'sg'], w=['tmp'])
                              kb.op('pool', lambda e, ns=ns: e.tensor_tensor(out=u[:, ns], in0=u[:, ns], in1=tmp[:], op=ALU.add), r=['u', 'tmp'], w=['u'])
                          for hb in range(2):
                              kb.mm([lambda e, k=k, hb=hb: e.transpose(ptp[hb][:, k % 4, :], u[:, k * 128:(k + 1) * 128], ident[:]) for k in range(hb * 4, hb * 4 + 4)], r=['u', 'ident'], w=[('ptp', hb)])
                              kb.op('act', lambda e, hb=hb: e.copy(out=uT[:, hb * 4:hb * 4 + 4, :], in_=ptp[hb][:]), r=[('ptp', hb)], w=['uT'])
                          for n in range(2):
                              kb.mm([lambda e, k=k, n=n: e.matmul(pyl[n][:, :], lhsT=uT[:, k, :], rhs=wo[:, k, n * 512:(n + 1) * 512], start=(k == 0), stop=(k == 7)) for k in range(8)], r=['uT', 'wo'], w=[('pyl', n)])
                              kb.op('dve', lambda e, n=n, j=j: e.tensor_tensor(out=rr[:, n * 512:(n + 1) * 512], in0=pyl[n][:, :], in1=modrow[:, j, 0, n * 512:(n + 1) * 512], op=ALU.mult), r=[('pyl', n), 'modrow'], w=['rr'])
                          kb.op('dve', lambda e, s=s: e.scalar_tensor_tensor(out=rr[:], in0=xr[s][:], scalar=ALPHA, in1=rr[:], op0=ALU.mult, op1=ALU.add), r=[('xr', s), 'rr'], w=['rr'])
                          layer_norm(rr, 'rr', x1, 'x1')
                          kb.op('pool', lambda e: e.tensor_tensor(out=x1[:], in0=x1[:], in1=g1r[:], op=ALU.mult), r=['x1', 'g1r'], w=['x1'])
                          kb.op('pool', lambda e: e.tensor_tensor(out=x1[:], in0=x1[:], in1=b1r[:], op=ALU.add), r=['x1', 'b1r'], w=['x1'])
                          kb.dma('sp', X1[tsl, :], x1[:], 'x1o', r=['x1'], w=[K('X1')])
                          layer_norm(x1, 'x1', xn2, 'xn2')
                          for hb in range(2):
                              kb.mm([lambda e, k=k, hb=hb: e.transpose(ptp[hb][:, k % 4, :], xn2[:, k * 128:(k + 1) * 128], ident[:]) for k in range(hb * 4, hb * 4 + 4)], r=['xn2', 'ident'], w=[('ptp', hb)])
                              for k in range(hb * 4, hb * 4 + 4):
                                  kb.op('act', lambda e, k=k, hb=hb, j=j: e.activation(out=h2f[:, k, :], in_=ptp[hb][:, k % 4, :], func=AF.Identity,
                                                                                       bias=modcol[:, j, 2, k:k + 1], scale=modcol[:, j, 3, k:k + 1]), r=[('ptp', hb), 'modcol'], w=['h2f'])
                          kb.op('pool', lambda e: e.tensor_copy(out=h2b[:], in_=h2f[:]), r=['h2f'], w=['h2b'])
                          kb.dma('sp', H2T[:, :, tsl], h2b[:], 'h2o', r=['h2b'], w=[K('H2T')])
                          kb.mm([lambda e, k=k: e.matmul(prt[:, :], lhsT=h2f[:, k, :], rhs=wr[:, k, :], start=(k == 0), stop=(k == 7)) for k in range(8)], r=['h2f', 'wr'], w=['prt'])
                          kb.op('dve', lambda e, tt=tt: e.tensor_tensor(out=logits[:, tt, :], in0=prt[:, :], in1=brr[:], op=ALU.add), r=['prt', 'brr'], w=['logits'])

              _chk('E')
              t0 = 0 if need_ctx else NCT
              NTE = NT - t0
              with scope() as pes:
                  with scope() as r1:
                      lg = logits[:, t0:NT, :]
                      gmax = sb(r1, [128, NTE], F32, "gmax")
                      gex = sb(r1, [128, NTE, 4], F32, "gex")
                      gsum = sb(r1, [128, NTE], F32, "gsum")
                      ohg = sb(r1, [128, NTE, 4], F32, "ohg")
                      elm = sb(r1, [128, NTE, 4, 8], F32, "elm")
                      els = sb(r1, [128, NTE, 8], F32, "els")
                      m1 = sb(r1, [128, NTE], F32, "m1")
                      m2 = sb(r1, [128, NTE], F32, "m2")
                      oh1 = sb(r1, [128, NTE, 8], F32, "oh1")
                      oh2 = sb(r1, [128, NTE, 8], F32, "oh2")
                      el2 = sb(r1, [128, NTE, 8], F32, "el2")
                      w1 = sb(r1, [128, NTE], F32, "w1")
                      w2 = sb(r1, [128, NTE], F32, "w2")
                      we = sb(r1, [128, NTE, 8], F32, "we")
                      R = 'rt'

                      def v(fn, r=(), w=()):
                          kb.op('dve', fn, r=[R] + list(r), w=[R] + list(w))
                      gl = lg[:, :, 0:4]
                      el = lg[:, :, 4:36].rearrange("p n (g j) -> p n g j", g=4)
                      v(lambda e: e.tensor_reduce(out=gmax[:], in_=gl, axis=AX.X, op=ALU.max), r=['logits'])
                      v(lambda e: e.tensor_tensor(out=gex[:], in0=gl, in1=gmax[:, :].unsqueeze(2).to_broadcast([128, NTE, 4]), op=ALU.subtract), r=['logits'])
                      v(lambda e: e.tensor_tensor(out=ohg[:], in0=gl, in1=gmax[:, :].unsqueeze(2).to_broadcast([128, NTE, 4]), op=ALU.is_ge), r=['logits'])
                      kb.op('act', lambda e: e.activation(out=gex[:], in_=gex[:], func=AF.Exp), r=[R], w=[R])
                      v(lambda e: e.tensor_reduce(out=gsum[:], in_=gex[:], axis=AX.X, op=ALU.add))
                      v(lambda e: e.reciprocal(out=gsum[:], in_=gsum[:]))
                      v(lambda e: e.tensor_tensor(out=elm[:], in0=el, in1=ohg[:].unsqueeze(3).to_broadcast([128, NTE, 4, 8]), op=ALU.mult), r=['logits'])
                      v(lambda e: e.tensor_reduce(out=els[:], in_=elm[:].rearrange("p n g j -> p n j g"), axis=AX.X, op=ALU.add))
                      v(lambda e: e.tensor_reduce(out=m1[:], in_=els[:], axis=AX.X, op=ALU.max))
                      v(lambda e: e.tensor_tensor(out=oh1[:], in0=els[:], in1=m1[:, :].unsqueeze(2).to_broadcast([128, NTE, 8]), op=ALU.is_ge))
                      v(lambda e: e.scalar_tensor_tensor(out=el2[:], in0=oh1[:], scalar=-1e30, in1=els[:], op0=ALU.mult, op1=ALU.add))
                      v(lambda e: e.tensor_reduce(out=m2[:], in_=el2[:], axis=AX.X, op=ALU.max))
                      v(lambda e: e.tensor_tensor(out=oh2[:], in0=el2[:], in1=m2[:, :].unsqueeze(2).to_broadcast([128, NTE, 8]), op=ALU.is_ge))
                      v(lambda e: e.tensor_tensor(out=w2[:], in0=m2[:], in1=m1[:], op=ALU.subtract))
                      kb.op('act', lambda e: e.activation(out=w2[:], in_=w2[:], func=AF.Exp), r=[R], w=[R])
                      v(lambda e: e.tensor_scalar(out=w1[:], in0=w2[:], scalar1=1.0, scalar2=None, op0=ALU.add))
                      v(lambda e: e.reciprocal(out=w1[:], in_=w1[:]))
                      v(lambda e: e.tensor_tensor(out=w2[:], in0=w2[:], in1=w1[:], op=ALU.mult))
                      v(lambda e: e.tensor_tensor(out=w1[:], in0=w1[:], in1=gsum[:], op=ALU.mult))
                      v(lambda e: e.tensor_tensor(out=w2[:], in0=w2[:], in1=gsum[:], op=ALU.mult))
                      v(lambda e: e.tensor_tensor(out=we[:], in0=oh1[:], in1=w1[:, :].unsqueeze(2).to_broadcast([128, NTE, 8]), op=ALU.mult))
                      v(lambda e: e.tensor_tensor(out=oh2[:], in0=oh2[:], in1=w2[:, :].unsqueeze(2).to_broadcast([128, NTE, 8]), op=ALU.mult))
                      v(lambda e: e.tensor_tensor(out=we[:], in0=we[:], in1=oh2[:], op=ALU.add))
                      v(lambda e: e.tensor_tensor(out=wts[:, t0:NT, :].rearrange("p n (g j) -> p n g j", g=4), in0=ohg[:].unsqueeze(3).to_broadcast([128, NTE, 4, 8]),
                                                  in1=we[:].unsqueeze(2).to_broadcast([128, NTE, 4, 8]), op=ALU.mult), w=['wts'])
                  g2r = sb(pes, [128, D], F32, "g2r")
                  b2r = sb(pes, [128, D], F32, "b2r")
                  kb.dma('sp', g2r[:], ln2_g[l].partition_broadcast(128), K('ew2'), w=['g2r'])
                  kb.dma('sp', b2r[:], ln2_b[l].partition_broadcast(128), K('ew2'), w=['b2r'])
                  nh = (NTE + 1) // 2
                  halves = [(t0, t0 + nh), (t0 + nh, NT)]
                  wei = [sb(pes, [128, 8, D], BF16, "wei") for _ in range(2)]
                  weo = [sb(pes, [128, 4, D], BF16, "weo") for _ in range(2)]
                  h2 = sb(pes, [128, 8, nh * 128], BF16, "h2")
                  acc = sb(pes, [128, nh, D], F32, "acc")
                  sgt = sb(pes, [128, 512], F32, "sgt")
                  aT = [sb(pes, [128, 4, 512], BF16, "aT")] * 2
                  xr = [sb(pes, [128, D], F32, "xr2")] * 2
                  rr = sb(pes, [128, D], F32, "rr2")
                  xo = [sb(pes, [128, D], F32, "xo")] * 2
                  st = sb(pes, [128, 2, 6], F32, "st2")
                  mv = sb(pes, [128, 2], F32, "mv2")
                  rstd = sb(pes, [128, 1], F32, "rstd2")
                  pgu = [ps(pes, [128, 512], F32, "pgu") for _ in range(4)]
                  po = [ps(pes, [128, 512], F32, "po") for _ in range(4)]
                  ei = 0
                  for (ha, hb_) in halves:
                      nt_h = hb_ - ha
                      ntok = nt_h * 128
                      kb.dma('sp', h2[:, :, 0:ntok], H2T[:, :, ha * 128:hb_ * 128], 'h2l', r=[K('H2T')], w=['h2'])
                      kb.op('pool', lambda e: e.memset(acc[:].rearrange("p n d -> p (n d)"), 0.0), w=['acc'])
                      groups = [(a, min(a + 512, ntok)) for a in range(0, ntok, 512)]
                      for ex_ in range(NEXP):
                          s = ei % 2
                          ei += 1
                          kb.dma('pool', wei[s][:], w_ei[l, ex_ % nexp_decl].rearrange("(k p) n -> p k n", p=128), ('wei', s), w=[('wei', s)])
                          kb.dma('pool', weo[s][:], w_eo[l, ex_ % nexp_decl].rearrange("(k p) n -> p k n", p=128), ('weo', s), w=[('weo', s)])
                          for gi, (a, b) in enumerate(groups):
                              n = b - a
                              at = aT[gi % 2]
                              for jc in range(4):
                                  pg, pu = pgu[(jc % 2) * 2], pgu[(jc % 2) * 2 + 1]
                                  kb.mm([lambda e, k=k, jc=jc, pg=pg, s=s: e.matmul(pg[:, 0:n], lhsT=wei[s][:, k, jc * 128:(jc + 1) * 128], rhs=h2[:, k, a:b], start=(k == 0), stop=(k == 7)) for k in range(8)],
                                        r=[('wei', s), 'h2'], w=[('pgu', (jc % 2) * 2)])
                                  kb.mm([lambda e, k=k, jc=jc, pu=pu, s=s: e.matmul(pu[:, 0:n], lhsT=wei[s][:, k, 512 + jc * 128:512 + (jc + 1) * 128], rhs=h2[:, k, a:b], start=(k == 0), stop=(k == 7)) for k in range(8)],
                                        r=[('wei', s), 'h2'], w=[('pgu', (jc % 2) * 2 + 1)])
                                  kb.op('act', lambda e, pg=pg: e.activation(out=sgt[:, 0:n], in_=pg[:, 0:n], func=AF.Silu), r=[('pgu', (jc % 2) * 2)], w=['sgt'])
                                  kb.op('dve', lambda e, pu=pu, jc=jc, at=at: e.tensor_tensor(out=at[:, jc, 0:n], in0=pu[:, 0:n], in1=sgt[:, 0:n], op=ALU.mult),
                                        r=[('pgu', (jc % 2) * 2 + 1), 'sgt'], w=[('aT', 0)])
                              for ti in range(a // 128, b // 128):
                                  tloc = ti * 128 - a
                                  for n2 in range(2):
                                      pb_ = po[(ti % 2) * 2 + n2]
                                      kb.mm([lambda e, k=k, pb_=pb_, tloc=tloc, n2=n2, at=at, s=s: e.matmul(pb_[:, :], lhsT=at[:, k, tloc:tloc + 128], rhs=weo[s][:, k, n2 * 512:(n2 + 1) * 512], start=(k == 0), stop=(k == 3)) for k in range(4)],
                                            r=[('aT', 0), ('weo', s)], w=[('po', (ti % 2) * 2 + n2)])
                                      kb.op('dve', lambda e, pb_=pb_, ti=ti, n2=n2, ex_=ex_: e.scalar_tensor_tensor(
                                          out=acc[:, ti, n2 * 512:(n2 + 1) * 512], in0=pb_[:, :], scalar=wts[:, ha + ti, ex_:ex_ + 1], in1=acc[:, ti, n2 * 512:(n2 + 1) * 512], op0=ALU.mult, op1=ALU.add),
                                          r=[('po', (ti % 2) * 2 + n2), 'wts', 'acc'], w=['acc'])
                      for ti in range(nt_h):
                          tt = ha + ti
                          s = tt % 2
                          j = 1 if tt < NCT else 0
                          tsl = slice(tt * 128, (tt + 1) * 128)
                          kb.dma('sp', xr[s][:], X1[tsl, :], ('xr2', 0), r=[K('X1')], w=[('xr2', 0)])
                          kb.op('pool', lambda e, ti=ti, j=j: e.tensor_tensor(out=rr[:], in0=acc[:, ti, :], in1=modrow[:, j, 1, :], op=ALU.mult), r=['acc', 'modrow'], w=['rr2'])
                          kb.op('dve', lambda e, s=s: e.scalar_tensor_tensor(out=rr[:], in0=xr[s][:], scalar=ALPHA, in1=rr[:], op0=ALU.mult, op1=ALU.add), r=[('xr2', 0), 'rr2'], w=['rr2'])
                          for h in range(2):
                              kb.op('dve', lambda e, h=h: e.bn_stats(out=st[:, h, :], in_=rr[:, h * 512:(h + 1) * 512]), r=['rr2'], w=['st2'])
                          kb.op('dve', lambda e: e.bn_aggr(out=mv[:], in_=st[:].rearrange("p a b -> p (a b)")), r=['st2'], w=['mv2'])
                          kb.op('act', lambda e: e.activation(out=rstd[:], in_=mv[:, 1:2], func=AF.Sqrt, bias=epsT[:, 0:1]), r=['mv2', 'epsT'], w=['rstd2'])
                          kb.op('dve', lambda e: e.reciprocal(out=rstd[:], in_=rstd[:]), r=['rstd2'], w=['rstd2'])
                          kb.op('dve', lambda e, s=s: e.tensor_scalar(out=xo[s][:], in0=rr[:], scalar1=mv[:, 0:1], scalar2=rstd[:, 0:1], op0=ALU.subtract, op1=ALU.mult),
                                r=['rr2', 'mv2', 'rstd2'], w=[('xo', 0)])
                          kb.op('pool', lambda e, s=s: e.tensor_tensor(out=xo[s][:], in0=xo[s][:], in1=g2r[:], op=ALU.mult), r=[('xo', 0), 'g2r'], w=[('xo', 0)])
                          kb.op('pool', lambda e, s=s: e.tensor_tensor(out=xo[s][:], in0=xo[s][:], in1=b2r[:], op=ALU.add), r=[('xo', 0), 'b2r'], w=[('xo', 0)])
                          if l == DEPTH - 1:
                              kb.dma('sp', y_out[(tt - NCT) * 128:(tt - NCT + 1) * 128, :], xo[s][:], ('xoo', 0), r=[('xo', 0)], w=['yout'])
                          else:
                              kb.dma('sp', X_out[tsl, :], xo[s][:], ('xoo', 0), r=[('xo', 0)], w=[('X2w', l + 1)])

        except _Stop:
            pass
        kb.mute = False
        kb.flush('sp')
    return nc


_PF, _PT = _perm_cols()


def _prep_shared(inp, NLT):
    f32 = np.float32
    w_in = np.asarray(inp['w_in'], f32)
    sh = {
        'w_ada': np.ascontiguousarray(inp['w_ada'], f32),
        'b_ada': np.ascontiguousarray(inp['b_ada'], f32),
        'w_f': np.ascontiguousarray(w_in[:, :, _PF]),
        'w_t': np.ascontiguousarray(w_in[:, :, _PT]),
        'conv_w': np.ascontiguousarray(np.asarray(inp['conv_w'], f32).reshape(DEPTH, 3, 2, 128).transpose(0, 3, 2, 1)),
        'sink': np.ascontiguousarray(inp['attn_sink'], f32),
        'gate_b': np.ascontiguousarray(inp['mlstm_gate_b'], f32),
        'norm_w': np.ascontiguousarray(inp['mlstm_norm_w'], f32),
        'w_pa': np.ascontiguousarray(inp['w_proj_a'], f32),
        'w_pb': np.ascontiguousarray(inp['w_proj_b'], f32),
        'w_pc': np.ascontiguousarray(inp['w_proj_c'], f32),
        'w_o': np.ascontiguousarray(inp['w_out'], f32),
        'ln1_g': np.ascontiguousarray(inp['ln1_g'], f32),
        'ln1_b': np.ascontiguousarray(inp['ln1_b'], f32),
        'ln2_g': np.ascontiguousarray(inp['ln2_g'], f32),
        'ln2_b': np.ascontiguousarray(inp['ln2_b'], f32),
        'w_r': np.ascontiguousarray(np.concatenate([inp['w_route_group'], inp['w_route_expert']], axis=-1), f32),
        'b_r': np.ascontiguousarray(np.concatenate([inp['b_route_group'], inp['b_route_expert']], axis=-1), f32),
        'w_ei': np.ascontiguousarray(inp['w_expert_in'], f32),
        'w_eo': np.ascontiguousarray(inp['w_expert_out'], f32),
    }
    for k, v in _consts(NLT).items():
        sh['c_' + k] = v
    return sh


def _prep_core(inp, b):
    f32 = np.float32
    m = {}
    m['xin'] = np.ascontiguousarray(np.concatenate([inp['ctx'][b], inp['x'][b]], axis=0), f32)
    cs = np.stack([np.asarray(inp['c'][b], f32), np.asarray(inp['c_ctx'], f32)], axis=-1)
    m['cs'] = np.ascontiguousarray(cs.reshape(8, 128, 2).transpose(1, 0, 2))
    return m


def kernel(**inputs):
    inp = {k: np.asarray(v) for k, v in inputs.items()}
    B, L, _ = inp['x'].shape
    NLT = L // 128
    nc = build(NLT)
    sh = _prep_shared(inp, NLT)
    in_maps = []
    for b in range(B):
        m = dict(sh)
        m.update(_prep_core(inp, b))
        in_maps.append(m)
    res = run_bass_kernel_spmd(nc, in_maps, core_ids=list(range(B)))
    return np.stack([np.asarray(r['y'], np.float32) for r in res.results], axis=0)
```

```python
import os
import numpy as np
from contextlib import ExitStack, contextmanager
import concourse.bass as bass
import concourse.mybir as mybir
from concourse.bass_utils import run_bass_kernel_spmd

F32 = mybir.dt.float32
BF16 = mybir.dt.bfloat16
AF = mybir.ActivationFunctionType
ALU = mybir.AluOpType
AX = mybir.AxisListType

D = 1024
NCT = 2
DEPTH = 2
ALPHA = float((2 * DEPTH) ** 0.25)
LN_EPS = 1e-6
NEXP = 32


class KB:
    def __init__(self, nc, es):
        self.nc = nc
        self.es = es
        self.eng = {'pe': nc.tensor, 'act': nc.scalar, 'dve': nc.vector, 'pool': nc.gpsimd, 'sp': nc.sync}
        self.sem = {n: es.enter_context(nc.semaphore('s_' + n)) for n in self.eng}
        self.cnt = {n: 0 for n in self.eng}
        self.waited = {n: {} for n in self.eng}
        self.lastw = {}
        self.readers = {}
        self.chans = {}
        self.chan_by_sid = {}
        self.nsem = 0
        self.mute = False

    def _wait(self, e, tok):
        sid, sem, val, src = tok
        if src == e and e == 'pe':
            return
        if src == 'dma':
            val = self.chan_by_sid[sid][1]
        w = self.waited[e]
        if w.get(sid, 0) >= val:
            return
        w[sid] = val
        self.eng[e].wait_ge(sem, val)

    def _deps(self, e, r, w):
        toks = []
        for k in r:
            if k in self.lastw:
                toks.append(self.lastw[k])
        for k in w:
            if k in self.lastw:
                toks.append(self.lastw[k])
            toks.extend(self.readers.get(k, {}).values())
        for t in toks:
            self._wait(e, t)

    def _commit(self, tok, r, w):
        for k in r:
            self.readers.setdefault(k, {})[tok[0]] = tok
        for k in w:
            self.lastw[k] = tok
            self.readers[k] = {}

    def op(self, e, fn, r=(), w=()):
        if self.mute:
            return None
        if e == 'pool':
            e = 'dve'
        self._deps(e, r, w)
        ins = fn(self.eng[e])
        self.cnt[e] += 1
        ins.then_inc(self.sem[e], 1)
        tok = ('e_' + e, self.sem[e], self.cnt[e], e)
        self._commit(tok, r, w)
        return tok

    def mm(self, fns, r=(), w=(), drain=False):
        if self.mute:
            return None
        self._deps('pe', r, w)
        if drain and self.cnt['pe'] > 0 and self.waited['pe'].get('e_pe', 0) < self.cnt['pe']:
            self.waited['pe']['e_pe'] = self.cnt['pe']
            self.nc.tensor.wait_ge(self.sem['pe'], self.cnt['pe'])
        ins = None
        for fn in fns:
            ins = fn(self.nc.tensor)
        self.cnt['pe'] += 1
        ins.then_inc(self.sem['pe'], 1)
        tok = ('e_pe', self.sem['pe'], self.cnt['pe'], 'pe')
        self._commit(tok, r, w)
        return tok

    def dma(self, q, out, in_, chan, r=(), w=()):
        if self.mute:
            return None
        self._deps(q, r, w)
        if chan not in self.chans:
            self.chans[chan] = [self.es.enter_context(self.nc.semaphore('d%d' % len(self.chans))), 0]
        c = self.chans[chan]
        self.chan_by_sid['c_%s' % (chan,)] = c
        c[1] += 16
        self.eng[q].dma_start(out=out, in_=in_).then_inc(c[0], 16)
        tok = ('c_%s' % (chan,), c[0], c[1], 'dma')
        self._commit(tok, r, w)
        return tok

    def barrier(self):
        if self.mute:
            return
        toks = [('e_' + n, self.sem[n], self.cnt[n], n) for n in self.eng if self.cnt[n] > 0]
        toks += [('c_%s' % (ch,), c[0], c[1], 'dma') for ch, c in self.chans.items() if c[1] > 0]
        for e in self.eng:
            for t in toks:
                if t[3] == e:
                    continue
                w = self.waited[e]
                if w.get(t[0], 0) >= t[2]:
                    continue
                w[t[0]] = t[2]
                self.eng[e].wait_ge(t[1], t[2])

    def flush(self, e='sp'):
        for chan, c in self.chans.items():
            if c[1] > 0:
                self._wait(e, ('c_%s' % (chan,), c[0], c[1], 'dma'))


def _perm_cols():
    o_bg, o_cg, o_xin, o_q, o_k, o_v, o_mq, o_mk, o_mv, o_og, o_gt, o_mg = (
        0, 256, 512, 768, 1280, 1408, 1536, 1792, 2048, 2304, 2560, 2576)
    part = np.array([d + 16 if (d % 32) < 16 else d - 16 for d in range(64)])
    f = []
    f += list(range(o_bg, o_bg + 256)) + list(range(o_cg, o_cg + 256)) + list(range(o_xin, o_xin + 256))
    for c in range(4):
        f += [o_q + c * 64 + d for d in range(64)] + [o_q + (4 + c) * 64 + d for d in range(64)]
    for c in range(4):
        f += [o_q + c * 64 + part[d] for d in range(64)] + [o_q + (4 + c) * 64 + part[d] for d in range(64)]
    f += [o_k + d for d in range(128)]
    f += [o_k + h * 64 + part[d] for h in range(2) for d in range(64)]
    f += list(range(o_mq, o_mq + 256)) + list(range(o_mk, o_mk + 256))
    t = []
    t += list(range(o_v, o_v + 128)) + list(range(o_mk, o_mk + 256))
    t += list(range(o_mv, o_mv + 256)) + list(range(o_og, o_og + 256))
    t += list(range(o_mg, o_mg + 3072))
    t += list(range(o_gt, o_gt + 16))
    return np.array(f), np.array(t)


def _consts(NLT):
    L = NLT * 128
    nf = 16
    inv = 10000.0 ** (-np.arange(nf, dtype=np.float32) / nf)
    pos = np.arange(L)
    pr = (pos // 64).astype(np.float32)
    pc = (pos % 64).astype(np.float32)
    C = np.zeros((128, L), np.float32)
    S = np.zeros((128, L), np.float32)
    for p in range(128):
        d = p % 64
        posv = pr if d < 32 else pc
        fi = d % 16
        ang = posv * inv[fi]
        C[p] = np.cos(ang)
        S[p] = -np.sin(ang) if (d % 32) < 16 else np.sin(ang)
    i = np.arange(128)
    same = (i[:, None] // 64) == (i[None, :] // 64)
    le = i[:, None] <= i[None, :]
    ge = i[:, None] >= i[None, :]
    cm = np.zeros((128, 2, 128), np.float32)
    cm[:, 0, 0:64] = 1.0
    cm[:, 1, 64:128] = 1.0
    cmt = np.zeros((128, 2), np.float32)
    cmt[0:64, 0] = 1.0
    cmt[64:128, 1] = 1.0
    return {
        'ropeC': C, 'ropeS': S,
        'ident': np.eye(128, dtype=np.float32),
        'ones': np.ones((128, 128), np.float32),
        'tri0': (same & le).astype(np.float32),
        'tri1': (same & ge).astype(np.float32),
        'blk': same.astype(np.float32),
        'amprev': (i[None, :] <= i[:, None]).astype(np.float32),
        'amnext': (i[:, None] <= i[None, :]).astype(np.float32),
        'cm': cm, 'cmt': cmt,
    }


class _Stop(Exception):
    pass


def build(NLT, debug=False, stop=DEPTH, upto=None, nexp_decl=NEXP, only=None, cpt=None):
    NT = NCT + NLT
    T = NT * 128
    L = NLT * 128
    nc = bass.Bass("TRN2", target_bir_lowering=False)
    dbg_kind = "ExternalOutput" if debug else "Internal"

    BIG = ('w_ada', 'w_f', 'w_t', 'w_ei', 'w_eo', 'w_pa', 'w_pb', 'w_pc', 'w_o', 'xin')

    def din(name, shape, dt=F32):
        kind = "Internal" if (only is not None and name in BIG) else "ExternalInput"
        return nc.dram_tensor(name, list(shape), dt, kind=kind).ap()

    def dscr(name, shape, dt):
        return nc.dram_tensor(name, list(shape), dt, kind=dbg_kind).ap()

    xin = din("xin", [T, D])
    cs_d = din("cs", [128, 8, 2])
    w_ada = din("w_ada", [DEPTH, D, 6 * D])
    b_ada = din("b_ada", [DEPTH, 6 * D])
    w_f = din("w_f", [DEPTH, D, 2560])
    w_t = din("w_t", [DEPTH, D, 3984])
    conv_w = din("conv_w", [DEPTH, 128, 2, 3])
    sink = din("sink", [DEPTH, 8])
    gate_b = din("gate_b", [DEPTH, 16])
    norm_w = din("norm_w", [DEPTH, 256])
    w_pa = din("w_pa", [DEPTH, 256, D])
    w_pb = din("w_pb", [DEPTH, 512, D])
    w_pc = din("w_pc", [DEPTH, 256, D])
    w_o = din("w_o", [DEPTH, D, D])
    ln1_g = din("ln1_g", [DEPTH, D])
    ln1_b = din("ln1_b", [DEPTH, D])
    ln2_g = din("ln2_g", [DEPTH, D])
    ln2_b = din("ln2_b", [DEPTH, D])
    w_r = din("w_r", [DEPTH, D, 36])
    b_r = din("b_r", [DEPTH, 36])
    w_ei = din("w_ei", [DEPTH, nexp_decl, D, D])
    w_eo = din("w_eo", [DEPTH, nexp_decl, 512, D])
    cst = {k: din("c_" + k, v.shape) for k, v in _consts(NLT).items()}
    y_out = nc.dram_tensor("y", [L, D], F32, kind="ExternalOutput").ap()

    ZF = dscr("ZF", [20, 128, T], BF16)
    ZT = dscr("ZT", [T, 3968], BF16)
    X1 = dscr("X1", [T, D], F32)
    X2 = dscr("X2", [T, D], F32)
    H2T = dscr("H2T", [128, 8, T], BF16)
    HFD = dscr("HFD", [T, 256], F32)
    DBA = dscr("DBA", [128, 2, T], BF16) if debug else None
    DBB = dscr("DBB", [128, 4, T], BF16) if debug else None
    DBC = dscr("DBC", [128, 2, T], BF16) if debug else None

    with ExitStack() as es:
        kb = KB(nc, es)

        @contextmanager
        def scope():
            stopped = False
            with ExitStack() as s_:
                try:
                    yield s_
                except _Stop:
                    stopped = True
            if stopped:
                raise _Stop()
            kb.barrier()
        uid = [0]

        def sb(es_, shape, dt=F32, name=None):
            uid[0] += 1
            return es_.enter_context(nc.sbuf_tensor((name or "t") + "_%d" % uid[0], list(shape), dt))

        def ps(es_, shape, dt=F32, name=None):
            uid[0] += 1
            n = int(np.prod(shape[1:]))
            assert n <= 512 and dt == F32
            t = es_.enter_context(nc.psum_tensor((name or "p") + "_%d" % uid[0], [128, 512], F32))
            v = t[:, 0:n]
            if len(shape) == 3:
                v = v.rearrange("p (a b) -> p a b", a=shape[1])
            return v

        ident = sb(es, [128, 128], F32, "ident")
        ones = sb(es, [128, 128], F32, "ones")
        kb.dma('sp', ident[:], cst['ident'][:, :], 'cst', w=['ident'])
        kb.dma('sp', ones[:], cst['ones'][:, :], 'cst', w=['ones'])
        epsT = sb(es, [128, 1], F32, "epsT")
        kb.op('pool', lambda e: e.memset(epsT[:], LN_EPS), w=['epsT'])
        cs = sb(es, [128, 8, 2], F32, "cs")
        ss = sb(es, [128, 8, 2], F32, "ss")
        kb.dma('sp', cs[:], cs_d[:, :, :], 'cst', w=['cs'])
        kb.op('act', lambda e: e.activation(out=ss[:], in_=cs[:], func=AF.Silu), r=['cs'], w=['ss'])
        modcol = sb(es, [128, 2, 4, 8], F32, "modcol")
        modrow = sb(es, [128, 2, 2, D], F32, "modrow")
        gates = sb(es, [128, NT, 16], F32, "gates")
        logits = sb(es, [128, NT, 36], F32, "logits")
        wts = sb(es, [128, NT, 32], F32, "wts")

        order_ = ['M', 'P', 'A', 'B', 'C', 'D', 'E']

        def _chk(ph):
            if upto is not None and order_.index(ph) > order_.index(upto):
                raise _Stop()
            kb.mute = (only is not None and ph not in only)

        def _pt(n):
            if cpt is not None and n > cpt:
                raise _Stop()

        try:
          for l in range(DEPTH):
              if l >= stop:
                  break
              need_ctx = l < DEPTH - 1
              X_in = xin if l == 0 else X2
              X_out = X2

              def K(name):
                  return (name, l)

              _chk('M')
              with scope() as pes:
                  modrep = sb(pes, [128, 2, 6, D], F32, "modrep")
                  brep = sb(pes, [128, 6 * D], F32, "brep")
                  kb.dma('sp', brep[:], b_ada[l].partition_broadcast(128), K('brep'), w=[K('brep')])
                  for c in (1, 4):
                      kb.op('dve', lambda e, c=c: e.tensor_scalar_add(out=brep[:, c * D:(c + 1) * D], in0=brep[:, c * D:(c + 1) * D], scalar1=1.0),
                            r=[K('brep')], w=[K('brep')])
                  wa = [sb(pes, [128, 8, 512], F32, "wa") for _ in range(2)]
                  pm = [ps(pes, [128, 512], F32, "pm") for _ in range(2)]
                  pcol = ps(pes, [128, 64], F32, "pcol")
                  wsrc = w_ada[l].rearrange("(k p) n -> p k n", p=128)
                  for cg in range(12):
                      s = cg % 2
                      kb.dma('sp', wa[s][:], wsrc[:, :, cg * 512:(cg + 1) * 512], ('wa', s), w=[('wa', s)])
                      chunk, half = cg // 2, cg % 2
                      for j in range(2):
                          kb.mm([lambda e, k=k, j=j, s=s: e.matmul(pm[j][:, :], lhsT=ss[:, k, j:j + 1].to_broadcast([128, 128]), rhs=wa[s][:, k, :],
                                                                    start=(k == 0), stop=(k == 7)) for k in range(8)],
                                r=['ss', ('wa', s)], w=[('pm', j)])
                          kb.op('dve', lambda e, j=j, chunk=chunk, half=half, cg=cg: e.tensor_tensor(
                              out=modrep[:, j, chunk, half * 512:(half + 1) * 512], in0=pm[j][:, :], in1=brep[:, cg * 512:(cg + 1) * 512], op=ALU.add),
                              r=[('pm', j), K('brep')], w=[K('modrep')])
                  fns = []
                  for j in range(2):
                      for ci, c in enumerate((0, 1, 3, 4)):
                          for k in range(8):
                              idx = (j * 4 + ci) * 8 + k
                              fns.append(lambda e, j=j, c=c, k=k, idx=idx: e.matmul(
                                  pcol[:, idx:idx + 1], lhsT=modrep[:, j, c, k * 128:(k + 1) * 128], rhs=ident[:, 0:1], start=True, stop=True))
                  kb.mm(fns, r=[K('modrep'), 'ident'], w=['pcol'])
                  kb.op('dve', lambda e: e.tensor_copy(out=modcol[:].rearrange("p j c k -> p (j c k)"), in_=pcol[:, :]), r=['pcol'], w=['modcol'])
                  for j in range(2):
                      for gi, c in enumerate((2, 5)):
                          kb.op('dve', lambda e, j=j, gi=gi, c=c: e.tensor_copy(out=modrow[:, j, gi, :], in_=modrep[:, j, c, :]),
                                r=[K('modrep')], w=['modrow'])

              _chk('P')
              with scope() as pes:
                  hT = sb(pes, [128, 8, T], BF16, "hT")
                  with scope() as p1:
                      xt = [sb(p1, [128, D], F32, "xt") for _ in range(2)]
                      xn = [sb(p1, [128, D], F32, "xn") for _ in range(2)]
                      st = sb(p1, [128, 2, 6], F32, "st")
                      mv = sb(p1, [128, 2], F32, "mv")
                      rstd = sb(p1, [128, 1], F32, "rstd")
                      ptp = [ps(p1, [128, 4, 128], F32, "ptp") for _ in range(2)]
                      for tt in range(NT):
                          s = tt % 2
                          j = 1 if tt < NCT else 0
                          kb.dma('sp', xt[s][:], X_in[tt * 128:(tt + 1) * 128, :], ('xt', s), r=[K('X2w')] if l > 0 else [], w=[('xt', s)])
                          for h in range(2):
                              kb.op('dve', lambda e, h=h, s=s: e.bn_stats(out=st[:, h, :], in_=xt[s][:, h * 512:(h + 1) * 512]), r=[('xt', s)], w=['st'])
                          kb.op('dve', lambda e: e.bn_aggr(out=mv[:], in_=st[:].rearrange("p a b -> p (a b)")), r=['st'], w=['mv'])
                          kb.op('act', lambda e: e.activation(out=rstd[:], in_=mv[:, 1:2], func=AF.Sqrt, bias=epsT[:, 0:1]), r=['mv', 'epsT'], w=['rstd'])
                          kb.op('dve', lambda e: e.reciprocal(out=rstd[:], in_=rstd[:]), r=['rstd'], w=['rstd'])
                          kb.op('dve', lambda e, s=s: e.tensor_scalar(out=xn[s][:], in0=xt[s][:], scalar1=mv[:, 0:1], scalar2=rstd[:, 0:1], op0=ALU.subtract, op1=ALU.mult),
                                r=[('xt', s), 'mv', 'rstd'], w=[('xn', s)])
                          for hb in range(2):
                              kb.mm([lambda e, k=k, hb=hb, s=s: e.transpose(ptp[hb][:, k % 4, :], xn[s][:, k * 128:(k + 1) * 128], ident[:]) for k in range(hb * 4, hb * 4 + 4)],
                                    r=[('xn', s), 'ident'], w=[('ptp', hb)])
                              for k in range(hb * 4, hb * 4 + 4):
                                  kb.op('act', lambda e, k=k, hb=hb, j=j, tt=tt: e.activation(
                                      out=hT[:, k, tt * 128:(tt + 1) * 128], in_=ptp[hb][:, k % 4, :], func=AF.Identity,
                                      bias=modcol[:, j, 0, k:k + 1], scale=modcol[:, j, 1, k:k + 1]),
                                      r=[('ptp', hb), 'modcol'], w=[K('hT')])
                  with scope() as p2:
                      wf = [sb(p2, [128, 8, 512], BF16, "wf") for _ in range(2)]
                      zf = [sb(p2, [128, T], BF16, "zf") for _ in range(2)]
                      pz = [ps(p2, [128, 512], F32, "pz") for _ in range(4)]
                      wsrc = w_f[l].rearrange("(k p) n -> p k n", p=128)
                      tgroups = [(0, NCT * 128)] + [(NCT * 128 + g * 512, NCT * 128 + (g + 1) * 512) for g in range(L // 512)]
                      ev = 0
                      for jg in range(5):
                          s = jg % 2
                          kb.dma('pool', wf[s][:], wsrc[:, :, jg * 512:(jg + 1) * 512], ('wf', s), w=[('wf', s)])
                          for jj in range(4):
                              jch = jg * 4 + jj
                              zs = jch % 2
                              for (a, b) in tgroups:
                                  pb_ = ev % 4
                                  kb.mm([lambda e, k=k, s=s, jj=jj, a=a, b=b, pb_=pb_: e.matmul(pz[pb_][:, 0:b - a], lhsT=wf[s][:, k, jj * 128:(jj + 1) * 128], rhs=hT[:, k, a:b],
                                                                                          start=(k == 0), stop=(k == 7)) for k in range(8)],
                                        r=[('wf', s), K('hT')], w=[('pz', pb_)])
                                  eng = 'act' if ev % 2 == 0 else 'dve'
                                  if eng == 'act':
                                      kb.op('act', lambda e, zs=zs, a=a, b=b, pb_=pb_: e.copy(out=zf[zs][:, a:b], in_=pz[pb_][:, 0:b - a]), r=[('pz', pb_)], w=[('zf', zs)])
                                  else:
                                      kb.op('dve', lambda e, zs=zs, a=a, b=b, pb_=pb_: e.tensor_copy(out=zf[zs][:, a:b], in_=pz[pb_][:, 0:b - a]), r=[('pz', pb_)], w=[('zf', zs)])
                                  ev += 1
                              kb.dma('sp', ZF[jch], zf[zs][:], ('zfo', zs), r=[('zf', zs)], w=[K('ZF')])
                  with scope() as p3:
                      wt = [sb(p3, [128, 8, 512], BF16, "wt") for _ in range(2)]
                      zt = [sb(p3, [128, 512], BF16, "zt") for _ in range(4)]
                      pz = [ps(p3, [128, 512], F32, "pz") for _ in range(4)]
                      wsrc = w_t[l].rearrange("(k p) n -> p k n", p=128)
                      cgroups = [(0, 384), (384, 896)] + [(896 + g * 512, 896 + (g + 1) * 512) for g in range(6)] + [(3968, 3984)]
                      ev = 0
                      for gi, (ca, cb) in enumerate(cgroups):
                          s = gi % 2
                          n = cb - ca
                          kb.dma('pool', wt[s][:, :, 0:n], wsrc[:, :, ca:cb], ('wt', s), w=[('wt', s)])
                          for tt in range(NT):
                              pb_ = ev % 4
                              kb.mm([lambda e, k=k, s=s, n=n, tt=tt, pb_=pb_: e.matmul(pz[pb_][:, 0:n], lhsT=hT[:, k, tt * 128:(tt + 1) * 128], rhs=wt[s][:, k, 0:n],
                                                                                        start=(k == 0), stop=(k == 7)) for k in range(8)],
                                    r=[('wt', s), K('hT')], w=[('pz3', pb_)])
                              if gi == 8:
                                  kb.op('dve', lambda e, tt=tt, pb_=pb_: e.tensor_copy(out=gates[:, tt, :], in_=pz[pb_][:, 0:16]), r=[('pz3', pb_)], w=['gates'])
                              else:
                                  zs = ev % 4
                                  if ev % 2 == 0:
                                      kb.op('act', lambda e, zs=zs, n=n, pb_=pb_: e.copy(out=zt[zs][:, 0:n], in_=pz[pb_][:, 0:n]), r=[('pz3', pb_)], w=[('zt', zs)])
                                  else:
                                      kb.op('dve', lambda e, zs=zs, n=n, pb_=pb_: e.tensor_copy(out=zt[zs][:, 0:n], in_=pz[pb_][:, 0:n]), r=[('pz3', pb_)], w=[('zt', zs)])
                                  kb.dma('sp', ZT[tt * 128:(tt + 1) * 128, ca:cb], zt[zs][:, 0:n], ('zto', zs), r=[('zt', zs)], w=[K('ZT')])
                              ev += 1

              with scope() as mes:
                  yaT = sb(mes, [128, 2, T], BF16, "yaT")
                  ybT = sb(mes, [128, 4, T], BF16, "ybT")
                  ycT = sb(mes, [128, 2, T], BF16, "ycT")

                  _chk('A')
                  with scope() as pes:
                      bg = sb(pes, [128, T], BF16, "bg")
                      cg_ = sb(pes, [128, T], BF16, "cg")
                      xi = sb(pes, [128, T], BF16, "xi")
                      P = sb(pes, [128, T], F32, "P")
                      acc = sb(pes, [128, T], F32, "acc")
                      cw = sb(pes, [128, 2, 3], F32, "cw")
                      kb.dma('sp', cw[:], conv_w[l], K('cw'), w=[K('cw')])
                      for c in range(2):
                          kb.dma('sp', bg[:], ZF[0 + c], 'cvl', r=[K('ZF')], w=['bg'])
                          kb.dma('sp', cg_[:], ZF[2 + c], 'cvl', r=[K('ZF')], w=['cg'])
                          kb.dma('sp', xi[:], ZF[4 + c], 'cvl', r=[K('ZF')], w=['xi'])
                          kb.op('dve', lambda e: e.tensor_tensor(out=P[:], in0=cg_[:], in1=xi[:], op=ALU.mult), r=['cg', 'xi'], w=['P'])
                          for (a, b) in ((0, NCT * 128), (NCT * 128, T)):
                              kb.op('dve', lambda e, a=a, b=b, c=c: e.tensor_scalar(out=acc[:, a:b], in0=P[:, a:b], scalar1=cw[:, c, 1:2], scalar2=None, op0=ALU.mult),
                                    r=['P', K('cw')], w=['acc'])
                              kb.op('dve', lambda e, a=a, b=b, c=c: e.scalar_tensor_tensor(out=acc[:, a + 1:b], in0=P[:, a:b - 1], scalar=cw[:, c, 0:1], in1=acc[:, a + 1:b],
                                                                                          op0=ALU.mult, op1=ALU.add), r=['P', K('cw')], w=['acc'])
                              kb.op('dve', lambda e, a=a, b=b, c=c: e.scalar_tensor_tensor(out=acc[:, a:b - 1], in0=P[:, a + 1:b], scalar=cw[:, c, 2:3], in1=acc[:, a:b - 1],
                                                                                          op0=ALU.mult, op1=ALU.add), r=['P', K('cw')], w=['acc'])
                          kb.op('dve', lambda e, c=c: e.tensor_tensor(out=yaT[:, c, :], in0=bg[:], in1=acc[:], op=ALU.mult), r=['bg', 'acc'], w=[K('yaT')])

                  _chk('B')
                  with scope() as pes:
                      qT = sb(pes, [128, 4, T], BF16, "qT")
                      kT = sb(pes, [128, T], BF16, "kT")
                      vext = sb(pes, [128, NT, 2, 65], BF16, "vext")
                      esink = sb(pes, [128, 8], F32, "esink")
                      mprev = sb(pes, [128, 128], BF16, "mprev")
                      mnext = sb(pes, [128, 128], BF16, "mnext")
                      kb.dma('pool', mprev[:], cst['amprev'][:, :], K('am'), w=['mprev'])
                      kb.dma('pool', mnext[:], cst['amnext'][:, :], K('am'), w=['mnext'])
                      kb.dma('sp', esink[:], sink[l].partition_broadcast(128), K('sink'), w=['esink'])
                      kb.op('act', lambda e: e.activation(out=esink[:], in_=esink[:], func=AF.Exp), r=['esink'], w=['esink'])
                      with scope() as r1:
                          RP = min(L, 1024)
                          rc = sb(r1, [128, RP], F32, "rc")
                          rs = sb(r1, [128, RP], F32, "rs")
                          qa = sb(r1, [128, T], BF16, "qa")
                          qb = sb(r1, [128, T], BF16, "qb")
                          t1 = sb(r1, [128, RP], F32, "t1")
                          t2 = sb(r1, [128, RP], F32, "t2")
                          vtmp = sb(r1, [128, NT, 128], BF16, "vtmp")
                          c0 = NCT * 128
                          for ci in range(5):
                              ja, jb = (6 + ci, 10 + ci) if ci < 4 else (14, 15)
                              kb.dma('sp', qa[:], ZF[ja], 'rpl', r=[K('ZF')], w=['qa'])
                              kb.dma('sp', qb[:], ZF[jb], 'rpl', r=[K('ZF')], w=['qb'])
                              dst = qT[:, ci, :] if ci < 4 else kT[:, :]
                              for pa in range(0, L, RP):
                                  kb.dma('sp', rc[:], cst['ropeC'][:, pa:pa + RP], K('rope'), w=['rc'])
                                  kb.dma('sp', rs[:], cst['ropeS'][:, pa:pa + RP], K('rope'), w=['rs'])
                                  kb.op('dve', lambda e: e.tensor_tensor(out=t1[:], in0=qa[:, c0 + pa:c0 + pa + RP], in1=rc[:], op=ALU.mult), r=['qa', 'rc'], w=['t1'])
                                  kb.op('dve', lambda e: e.tensor_tensor(out=t2[:], in0=qb[:, c0 + pa:c0 + pa + RP], in1=rs[:], op=ALU.mult), r=['qb', 'rs'], w=['t2'])
                                  kb.op('dve', lambda e, dst=dst: e.tensor_tensor(out=dst[:, c0 + pa:c0 + pa + RP], in0=t1[:], in1=t2[:], op=ALU.add), r=['t1', 't2'], w=[K('qkT')])
                              kb.op('dve', lambda e, dst=dst: e.tensor_copy(out=dst[:, 0:c0], in_=qa[:, 0:c0]), r=['qa'], w=[K('qkT')])
                          kb.dma('sp', vtmp[:], ZT[:, 0:128].rearrange("(n p) c -> p n c", p=128), 'rpl', r=[K('ZT')], w=['vtmp'])
                          kb.op('dve', lambda e: e.memset(vext[:].rearrange("p n g d -> p (n g d)"), 1.0), w=[K('vext')])
                          kb.op('dve', lambda e: e.tensor_copy(out=vext[:, :, :, 0:64], in_=vtmp[:].rearrange("p n (g d) -> p n g d", g=2)), r=['vtmp'], w=[K('vext')])
                      with scope() as r2:
                          pS = [ps(r2, [128, 4, 128], F32, "pS") for _ in range(3)]
                          ppv4 = [ps(r2, [128, 4, 65], F32, "ppv") for _ in range(4)]
                          ptr = ps(r2, [128, 4, 128], F32, "ptr")
                          eb = [sb(r2, [128, 4, 128], BF16, "eb") for _ in range(3)]
                          den = sb(r2, [128, 8], F32, "den")
                          yb = sb(r2, [128, 8, 64], F32, "yb")
                          qtiles = list(range(NT)) if need_ctx else list(range(NCT, NT))
                          it = 0
                          for qt in qtiles:
                              if qt < NCT:
                                  ktl = [(0, None), (1, None)]
                              else:
                                  ktl = [(0, None), (1, None)]
                                  if qt - 1 >= NCT:
                                      ktl.append((qt - 1, mprev))
                                  ktl.append((qt, None))
                                  if qt + 1 < NT:
                                      ktl.append((qt + 1, mnext))
                              qs = slice(qt * 128, (qt + 1) * 128)
                              qpar = qt % 2
                              ppv = ppv4[qpar * 2:qpar * 2 + 2]
                              for g in range(2):
                                  pr = slice(g * 64, (g + 1) * 64)
                                  for idx, (kt, msk) in enumerate(ktl):
                                      b3 = it % 3
                                      it += 1
                                      kb.mm([lambda e, b3=b3, pr=pr, kt=kt, qs=qs: e.matmul(pS[b3][:], lhsT=kT[pr, kt * 128:(kt + 1) * 128], rhs=qT[pr, :, qs], start=True, stop=True)],
                                            r=[K('qkT')], w=[('pS', b3)])
                                      kb.op('act', lambda e, b3=b3: e.activation(out=eb[b3][:], in_=pS[b3][:], func=AF.Exp, scale=0.125), r=[('pS', b3)], w=[('eb', b3)])
                                      if msk is not None:
                                          kb.op('pool', lambda e, b3=b3, msk=msk: e.tensor_tensor(out=eb[b3][:], in0=eb[b3][:], in1=msk[:, :].unsqueeze(1).to_broadcast([128, 4, 128]), op=ALU.mult),
                                                r=[('eb', b3), 'mprev', 'mnext'], w=[('eb', b3)])
                                      kb.mm([lambda e, b3=b3, c=c, g=g, kt=kt, idx=idx, n=len(ktl): e.matmul(ppv[g][:, c, :], lhsT=eb[b3][:, c, :], rhs=vext[:, kt, g, :],
                                                                                                         start=(idx == 0 and c == 0), stop=(idx == n - 1), skip_group_check=True) for c in range(4)],
                                            r=[('eb', b3), K('vext')], w=[('ppv', qpar, g)])
                              for g in range(2):
                                  kb.op('dve', lambda e, g=g: e.tensor_tensor(out=den[:, g * 4:(g + 1) * 4], in0=ppv[g][:, :, 64], in1=esink[:, g * 4:(g + 1) * 4], op=ALU.add),
                                        r=[('ppv', qpar, g), 'esink'], w=['den'])
                              kb.op('dve', lambda e: e.reciprocal(out=den[:], in_=den[:]), r=['den'], w=['den'])
                              for g in range(2):
                                  kb.op('dve', lambda e, g=g: e.tensor_tensor(out=yb[:, g * 4:(g + 1) * 4, :], in0=ppv[g][:, :, 0:64],
                                                                                in1=den[:, g * 4:(g + 1) * 4].unsqueeze(2).to_broadcast([128, 4, 64]), op=ALU.mult),
                                        r=[('ppv', qpar, g), 'den'], w=['yb'])
                              ybf = yb[:].rearrange("p h d -> p (h d)")
                              kb.mm([lambda e, c=c: e.transpose(ptr[:, c, :], ybf[:, c * 128:(c + 1) * 128], ident[:]) for c in range(4)], r=['yb', 'ident'], w=['ptr'])
                              kb.op('act', lambda e, qs=qs: e.copy(out=ybT[:, :, qs], in_=ptr[:]), r=['ptr'], w=[K('ybT')])

                  _chk('C')
                  with scope() as pes:
                      mqT = sb(pes, [128, 2, T], BF16, "mqT")
                      mkT = sb(pes, [128, 2, T], BF16, "mkT")
                      mkt = sb(pes, [128, NT, 256], BF16, "mkt")
                      vx = sb(pes, [128, NT, 4, 65], BF16, "vx")
                      LF = sb(pes, [128, NT, 2, 4], F32, "LF")
                      LI = sb(pes, [128, NT, 2, 4], F32, "LI")
                      tri = [sb(pes, [128, 128], F32, "tri") for _ in range(2)]
                      mskd = [sb(pes, [128, 128], F32, "mskd") for _ in range(2)]
                      blk = sb(pes, [128, 128], F32, "blk")
                      cm = sb(pes, [128, 2, 128], F32, "cm")
                      cmt = sb(pes, [128, 2], F32, "cmt")
                      gbr = sb(pes, [128, 16], F32, "gbr")
                      nwr = sb(pes, [128, 256], F32, "nwr")
                      for d_ in range(2):
                          kb.dma('sp', tri[d_][:], cst['tri%d' % d_][:, :], K('mc'), w=[('tri', d_)])
                          kb.op('pool', lambda e, d_=d_: e.tensor_scalar(out=mskd[d_][:], in0=tri[d_][:], scalar1=0.125, scalar2=None, op0=ALU.mult), r=[('tri', d_)], w=[('mskd', d_)])
                      kb.dma('sp', blk[:], cst['blk'][:, :], K('mc'), w=['blk'])
                      kb.dma('sp', cm[:], cst['cm'][:, :, :], K('mc'), w=['cm'])
                      kb.dma('sp', cmt[:], cst['cmt'][:, :], K('mc'), w=['cmt'])
                      kb.dma('sp', gbr[:], gate_b[l].partition_broadcast(128), K('mc'), w=['gbr'])
                      kb.dma('sp', nwr[:], norm_w[l].partition_broadcast(128), K('mc'), w=['nwr'])
                      for c in range(2):
                          kb.dma('sp', mqT[:, c, :], ZF[16 + c], K('mcl'), r=[K('ZF')], w=[K('mqT')])
                          kb.dma('sp', mkT[:, c, :], ZF[18 + c], K('mcl'), r=[K('ZF')], w=[K('mkT')])
                      kb.dma('sp', mkt[:], ZT[:, 128:384].rearrange("(n p) c -> p n c", p=128), K('mcl'), r=[K('ZT')], w=[K('mkt')])
                      _pt(0)
                      with scope() as r1:
                          vtmp = sb(r1, [128, NT, 256], BF16, "vtmp2")
                          ga = sb(r1, [128, NT, 16], F32, "ga")
                          ex = sb(r1, [128, NT, 2, 4], F32, "ex")
                          kb.dma('sp', vtmp[:], ZT[:, 384:640].rearrange("(n p) c -> p n c", p=128), K('mcl'), r=[K('ZT')], w=['vtmp2'])
                          kb.op('pool', lambda e: e.memset(vx[:].rearrange("p n h d -> p (n h d)"), 1.0), w=[K('vx')])
                          kb.op('pool', lambda e: e.tensor_copy(out=vx[:, :, :, 0:64], in_=vtmp[:].rearrange("p n (h d) -> p n h d", h=4)), r=['vtmp2'], w=[K('vx')])
                          kb.op('dve', lambda e: e.tensor_tensor(out=ga[:], in0=gates[:], in1=gbr[:, :].unsqueeze(1).to_broadcast([128, NT, 16]), op=ALU.add), r=['gates', 'gbr'], w=['ga'])
                          gav = ga[:].rearrange("p n (d t h) -> p n d t h", d=2, t=2)
                          kb.op('dve', lambda e: e.tensor_copy(out=LI[:], in_=gav[:, :, :, 0, :]), r=['ga'], w=[K('LI')])
                          kb.op('act', lambda e: e.activation(out=ex[:], in_=gav[:, :, :, 1, :], func=AF.Exp, scale=-1.0), r=['ga'], w=['ex'])
                          kb.op('act', lambda e: e.activation(out=ex[:], in_=ex[:], func=AF.Ln, bias=ones[:, 0:1]), r=['ex', 'ones'], w=['ex'])
                          kb.op('dve', lambda e: e.tensor_scalar(out=LF[:], in0=ex[:], scalar1=-1.0, scalar2=None, op0=ALU.mult), r=['ex'], w=[K('LF')])
                      _pt(1)
                      with scope() as r2:
                          psm = ps(r2, [128, 16], F32, "psm")
                          psg = ps(r2, [128, 16], F32, "psg")
                          pbr = ps(r2, [128, 4, 128], F32, "pbr")
                          pstb = [ps(r2, [128, 2, 128], F32, "pst") for _ in range(2)]
                          pkv = ps(r2, [128, 2, 130], F32, "pkv")
                          pnum = ps(r2, [128, 4, 65], F32, "pnum")
                          ptr = ps(r2, [128, 2, 128], F32, "ptrc")
                          Cst = sb(r2, [128, 2, 65], F32, "Cst")
                          CB = [sb(r2, [128, 2, 65], BF16, "CB") for _ in range(2)]
                          QZ = sb(r2, [128, 2, 2, 128], BF16, "QZ")
                          biasr = sb(r2, [128, 4], F32, "biasr")
                          utmp = sb(r2, [128, 4], F32, "utmp")
                          U = sb(r2, [128, 4], F32, "U")
                          lfm = sb(r2, [128, 2, 4], F32, "lfm")
                          EG = sb(r2, [128, 2, 4], F32, "EG")
                          DT = sb(r2, [128, 4, 128], F32, "DT")
                          EB = sb(r2, [128, 4, 128], F32, "EB")
                          EBM = sb(r2, [128, 4, 2, 128], F32, "EBM")
                          STM = sb(r2, [128, 4, 128], BF16, "STM")
                          Kp = sb(r2, [128, 4, 64], BF16, "Kp")
                          dn = sb(r2, [128, 4], F32, "dn")
                          hd = sb(r2, [128, 4, 64], F32, "hd")
                          mu = sb(r2, [128, 4], F32, "mu")
                          cen = sb(r2, [128, 4, 64], F32, "cen")
                          sq = sb(r2, [128, 4, 64], F32, "sq")
                          var = sb(r2, [128, 4], F32, "var")
                          ogt = sb(r2, [128, 256], BF16, "ogt")
                          hfl = sb(r2, [128, 256], F32, "hfl")
                          ogs = sb(r2, [128, 256], F32, "ogs")
                          yc = sb(r2, [128, 256], F32, "yc")
                          for d_ in range(2):
                              order = list(range(NT)) if d_ == 0 else (list(range(NCT - 1, -1, -1)) + list(range(NT - 1, NCT - 1, -1)))
                              chorder = (0, 1) if d_ == 0 else (1, 0)
                              kb.op('dve', lambda e: e.memset(Cst[:].rearrange("p c d -> p (c d)"), 0.0), w=['Cst'])
                              for tt in order:
                                  tsl = slice(tt * 128, (tt + 1) * 128)
                                  need_out = need_ctx or tt >= NCT
                                  lf_t = LF[:, tt, d_, :]
                                  kb.mm([lambda e: e.matmul(psm[:, 0:4], lhsT=tri[d_][:], rhs=lf_t, start=True, stop=True),
                                         lambda e: e.matmul(psm[:, 4:8], lhsT=blk[:], rhs=lf_t, start=True, stop=True)],
                                        r=[K('LF'), ('tri', d_), 'blk'], w=['psm'])
                                  kb.op('dve', lambda e: e.tensor_tensor(out=biasr[:], in0=LI[:, tt, d_, :], in1=psm[:, 0:4], op=ALU.subtract), r=[K('LI'), 'psm'], w=['biasr'])
                                  kb.op('dve', lambda e: e.tensor_tensor(out=utmp[:], in0=biasr[:], in1=psm[:, 4:8], op=ALU.add), r=['biasr', 'psm'], w=['utmp'])
                                  kb.op('act', lambda e: e.activation(out=U[:], in_=utmp[:], func=AF.Exp), r=['utmp'], w=['U'])
                                  _pt(2)
                                  kb.op('dve', lambda e: e.tensor_tensor(out=lfm[:], in0=lf_t.unsqueeze(1).to_broadcast([128, 2, 4]), in1=cmt[:, :].unsqueeze(2).to_broadcast([128, 2, 4]), op=ALU.mult),
                                        r=[K('LF'), 'cmt'], w=['lfm'])
                                  kb.mm([lambda e: e.matmul(psg[:, 0:8], lhsT=ones[:], rhs=lfm[:].rearrange("p a b -> p (a b)"), start=True, stop=True)], r=['lfm', 'ones'], w=['psm2'])
                                  kb.op('act', lambda e: e.activation(out=EG[:].rearrange("p a b -> p (a b)"), in_=psg[:, 0:8], func=AF.Exp), r=['psm2'], w=['EG'])
                                  _pt(3)
                                  if not os.environ.get('SKIP_PBR'):
                                      kb.mm([lambda e, h=h: e.matmul(pbr[:, h, :], lhsT=LF[:, tt, d_, h:h + 1].to_broadcast([128, 128]), rhs=tri[d_][:], start=True, stop=True) for h in range(4)],
                                            r=[K('LF'), ('tri', d_)], w=['pbr'])
                                  if not os.environ.get('SKIP_PST'):
                                    kb.mm([lambda e, h=h: e.matmul(pstb[h % 2][:, h // 2, :], lhsT=mkT[(h % 2) * 64:(h % 2) * 64 + 64, h // 2, tsl], rhs=mqT[(h % 2) * 64:(h % 2) * 64 + 64, h // 2, tsl], start=True, stop=True)
                                         for h in range(4)], r=[K('mkT'), K('mqT')], w=['pst'])
                                  for h in range(4):
                                      kb.op('act', lambda e, h=h: e.activation(out=DT[:, h, :], in_=pbr[:, h, :], func=AF.Exp, bias=biasr[:, h:h + 1]), r=['pbr', 'biasr'], w=['DT'])
                                  _pt(4)
                                  kb.op('act', lambda e: e.activation(out=EB[:], in_=pbr[:], func=AF.Exp), r=['pbr'], w=['EB'])
                                  kb.op('pool', lambda e: e.tensor_tensor(out=DT[:], in0=DT[:], in1=mskd[d_][:, :].unsqueeze(1).to_broadcast([128, 4, 128]), op=ALU.mult), r=['DT', ('mskd', d_)], w=['DT'])
                                  _pt(5)
                                  for half in range(2):
                                      kb.op('dve', lambda e, half=half: e.tensor_tensor(out=STM[:].rearrange("p (c x) d -> p c x d", x=2)[:, :, half, :], in0=pstb[half][:],
                                                                                            in1=DT[:].rearrange("p (c x) d -> p c x d", x=2)[:, :, half, :], op=ALU.mult), r=['pst', 'DT'], w=['STM'])
                                  kb.op('pool', lambda e: e.tensor_tensor(out=EBM[:], in0=EB[:].unsqueeze(2).to_broadcast([128, 4, 2, 128]), in1=cm[:].unsqueeze(1).to_broadcast([128, 4, 2, 128]), op=ALU.mult),
                                        r=['EB', 'cm'], w=['EBM'])
                                  for h in range(4):
                                      hr = slice((h % 2) * 64, (h % 2) * 64 + 64)
                                      kb.op('pool', lambda e, h=h, hr=hr: e.tensor_tensor(out=QZ[hr, h // 2, :, :], in0=mqT[hr, h // 2, tsl].unsqueeze(1).to_broadcast([64, 2, 128]), in1=EBM[hr, h, :, :], op=ALU.mult),
                                            r=[K('mqT'), 'EBM'], w=['QZ'])
                                  kb.op('dve', lambda e: e.scalar_tensor_tensor(out=Kp[:], in0=mkt[:, tt, :].rearrange("p (h d) -> p h d", h=4), scalar=0.125, in1=U[:, :].unsqueeze(2).to_broadcast([128, 4, 64]), op0=ALU.mult, op1=ALU.mult),
                                        r=[K('mkt'), 'U'], w=['Kp'])
                                  _pt(7)
                                  Kpf = Kp[:].rearrange("p h d -> p (h d)")
                                  for ci, ch in enumerate(chorder):
                                      kb.op('act', lambda e, ci=ci: e.copy(out=CB[ci][:], in_=Cst[:]), r=['Cst'], w=[('CB', ci)])
                                      cr = slice(ch * 64, ch * 64 + 64)
                                      kb.mm([lambda e, c=c, cr=cr: e.matmul(pkv[:, c, :], lhsT=Kpf[cr, c * 128:(c + 1) * 128], rhs=vx[cr, tt, 2 * c:2 * c + 2, :], start=True, stop=True) for c in range(2)],
                                            r=['Kp', K('vx')], w=['pkv'])
                                      for half in range(2):
                                          hr = slice(half * 64, half * 64 + 64)
                                          egb = EG[hr, ch, :].rearrange("p (c x) -> p c x", x=2)[:, :, half]
                                          kb.op('dve', lambda e, hr=hr, egb=egb: e.tensor_tensor(out=Cst[hr, :, :], in0=Cst[hr, :, :], in1=egb.unsqueeze(2).to_broadcast([64, 2, 65]), op=ALU.mult),
                                                r=['Cst', 'EG'], w=['Cst'])
                                          kb.op('dve', lambda e, hr=hr, half=half: e.tensor_tensor(out=Cst[hr, :, :], in0=Cst[hr, :, :], in1=pkv[hr, :, half * 65:(half + 1) * 65], op=ALU.add),
                                                r=['Cst', 'pkv'], w=['Cst'])
                                  _pt(8)
                                  if not need_out:
                                      continue
                                  fns = []
                                  for h in range(4):
                                      hr = slice((h % 2) * 64, (h % 2) * 64 + 64)
                                      fns.append(lambda e, h=h: e.matmul(pnum[:, h, :], lhsT=STM[:, h, :], rhs=vx[:, tt, h, :], start=True, stop=False))
                                      for ci, ch in enumerate(chorder):
                                          fns.append(lambda e, h=h, hr=hr, ci=ci, ch=ch: e.matmul(pnum[:, h, :], lhsT=QZ[hr, h // 2, ch, :], rhs=CB[ci][hr, h // 2, :], start=False, stop=(ci == 1)))
                                  kb.mm(fns, r=['STM', K('vx'), 'QZ', ('CB', 0), ('CB', 1)], w=['pnum'])
                                  _pt(9)
                                  kb.op('act', lambda e: e.activation(out=dn[:], in_=pnum[:, :, 64], func=AF.Abs), r=['pnum'], w=['dn'])
                                  kb.op('dve', lambda e: e.tensor_scalar(out=dn[:], in0=dn[:], scalar1=1.0, scalar2=None, op0=ALU.max), r=['dn'], w=['dn'])
                                  kb.op('dve', lambda e: e.reciprocal(out=dn[:], in_=dn[:]), r=['dn'], w=['dn'])
                                  if d_ == 0:
                                      kb.op('dve', lambda e: e.tensor_tensor(out=hd[:], in0=pnum[:, :, 0:64], in1=dn[:, :].unsqueeze(2).to_broadcast([128, 4, 64]), op=ALU.mult),
                                            r=['pnum', 'dn'], w=['hd'])
                                      kb.dma('sp', HFD[tsl, :], hd[:].rearrange("p h d -> p (h d)"), 'hfo', r=['hd'], w=[K('HFD')])
                                      continue
                                  kb.dma('sp', ogt[:], ZT[tsl, 640:896], 'ogl', r=[K('ZT')], w=['ogt'])
                                  kb.dma('sp', hfl[:], HFD[tsl, :], 'hfl', r=[K('HFD')], w=['hfl'])
                                  kb.op('dve', lambda e: e.tensor_tensor(out=hd[:], in0=pnum[:, :, 0:64], in1=dn[:, :].unsqueeze(2).to_broadcast([128, 4, 64]), op=ALU.mult), r=['pnum', 'dn'], w=['hd'])
                                  kb.op('dve', lambda e: e.tensor_tensor(out=hd[:], in0=hd[:], in1=hfl[:].rearrange("p (h d) -> p h d", h=4), op=ALU.add), r=['hd', 'hfl'], w=['hd'])
                                  kb.op('dve', lambda e: e.tensor_reduce(out=mu[:], in_=hd[:], axis=AX.X, op=ALU.add), r=['hd'], w=['mu'])
                                  kb.op('dve', lambda e: e.tensor_scalar(out=mu[:], in0=mu[:], scalar1=1.0 / 64, scalar2=None, op0=ALU.mult), r=['mu'], w=['mu'])
                                  kb.op('dve', lambda e: e.tensor_tensor(out=cen[:], in0=hd[:], in1=mu[:, :].unsqueeze(2).to_broadcast([128, 4, 64]), op=ALU.subtract), r=['hd', 'mu'], w=['cen'])
                                  kb.op('pool', lambda e: e.tensor_tensor(out=sq[:], in0=cen[:], in1=cen[:], op=ALU.mult), r=['cen'], w=['sq'])
                                  kb.op('dve', lambda e: e.tensor_reduce(out=var[:], in_=sq[:], axis=AX.X, op=ALU.add), r=['sq'], w=['var'])
                                  kb.op('act', lambda e: e.activation(out=var[:], in_=var[:], func=AF.Sqrt, bias=epsT[:, 0:1], scale=1.0 / 64), r=['var', 'epsT'], w=['var'])
                                  kb.op('dve', lambda e: e.reciprocal(out=var[:], in_=var[:]), r=['var'], w=['var'])
                                  kb.op('dve', lambda e: e.tensor_tensor(out=cen[:], in0=cen[:], in1=var[:, :].unsqueeze(2).to_broadcast([128, 4, 64]), op=ALU.mult), r=['cen', 'var'], w=['cen'])
                                  kb.op('act', lambda e: e.activation(out=ogs[:], in_=ogt[:], func=AF.Sigmoid), r=['ogt'], w=['ogs'])
                                  kb.op('pool', lambda e: e.tensor_tensor(out=ogs[:], in0=ogs[:], in1=nwr[:], op=ALU.mult), r=['ogs', 'nwr'], w=['ogs'])
                                  kb.op('dve', lambda e: e.tensor_tensor(out=yc[:], in0=cen[:].rearrange("p h d -> p (h d)"), in1=ogs[:], op=ALU.mult), r=['cen', 'ogs'], w=['yc'])
                                  kb.mm([lambda e, c=c: e.transpose(ptr[:, c, :], yc[:, c * 128:(c + 1) * 128], ident[:]) for c in range(2)], r=['yc', 'ident'], w=['ptrc'])
                                  kb.op('act', lambda e: e.copy(out=ycT[:, :, tsl], in_=ptr[:]), r=['ptrc'], w=[K('ycT')])

                  if debug and l == 0:
                      kb.dma('sp', DBA[:, :, :], yaT[:], 'dbg', r=[K('yaT')], w=['DBA'])
                      kb.dma('sp', DBB[:, :, :], ybT[:], 'dbg', r=[K('ybT')], w=['DBB'])
                      kb.dma('sp', DBC[:, :, :], ycT[:], 'dbg', r=[K('ycT')], w=['DBC'])
                  _chk('D')
                  with scope() as pes:
                      wpa = sb(pes, [128, 2, D], BF16, "wpa")
                      wpb = sb(pes, [128, 4, D], BF16, "wpb")
                      wpc = sb(pes, [128, 2, D], BF16, "wpc")
                      wo = sb(pes, [128, 8, D], BF16, "wo")
                      wr = sb(pes, [128, 8, 36], F32, "wr")
                      brr = sb(pes, [128, 36], F32, "brr")
                      g1r = sb(pes, [128, D], F32, "g1r")
                      b1r = sb(pes, [128, D], F32, "b1r")
                      kb.dma('pool', wpa[:], w_pa[l].rearrange("(k p) n -> p k n", p=128), K('dw'), w=['wpa'])
                      kb.dma('pool', wpb[:], w_pb[l].rearrange("(k p) n -> p k n", p=128), K('dw'), w=['wpb'])
                      kb.dma('pool', wpc[:], w_pc[l].rearrange("(k p) n -> p k n", p=128), K('dw'), w=['wpc'])
                      kb.dma('pool', wo[:], w_o[l].rearrange("(k p) n -> p k n", p=128), K('dw'), w=['wo'])
                      kb.dma('sp', wr[:], w_r[l].rearrange("(k p) n -> p k n", p=128), K('dw2'), w=['wr'])
                      kb.dma('sp', brr[:], b_r[l].partition_broadcast(128), K('dw2'), w=['brr'])
                      kb.dma('sp', g1r[:], ln1_g[l].partition_broadcast(128), K('dw2'), w=['g1r'])
                      kb.dma('sp', b1r[:], ln1_b[l].partition_broadcast(128), K('dw2'), w=['b1r'])
                      mg = [sb(pes, [128, 3072], BF16, "mg") for _ in range(2)]
                      sg = sb(pes, [128, 3072], F32, "sg")
                      xr = [sb(pes, [128, D], F32, "xr") for _ in range(2)]
                      u = sb(pes, [128, D], F32, "u")
                      tmp = sb(pes, [128, 512], F32, "tmp")
                      uT = sb(pes, [128, 8, 128], BF16, "uT")
                      rr = sb(pes, [128, D], F32, "rr")
                      x1 = sb(pes, [128, D], F32, "x1")
                      xn2 = sb(pes, [128, D], F32, "xn2")
                      h2f = sb(pes, [128, 8, 128], F32, "h2f")
                      h2b = sb(pes, [128, 8, 128], BF16, "h2b")
                      st = sb(pes, [128, 2, 6], F32, "st")
                      mv = sb(pes, [128, 2], F32, "mv")
                      rstd = sb(pes, [128, 1], F32, "rstd")
                      pabc = [ps(pes, [128, 512], F32, "pabc") for _ in range(3)]
                      ptp = [ps(pes, [128, 4, 128], F32, "ptp") for _ in range(2)]
                      pyl = [ps(pes, [128, 512], F32, "pyl") for _ in range(2)]
                      prt = ps(pes, [128, 36], F32, "prt")

                      def layer_norm(src, srckey, dst, dstkey):
                          for h in range(2):
                              kb.op('dve', lambda e, h=h: e.bn_stats(out=st[:, h, :], in_=src[:, h * 512:(h + 1) * 512]), r=[srckey], w=['st'])
                          kb.op('dve', lambda e: e.bn_aggr(out=mv[:], in_=st[:].rearrange("p a b -> p (a b)")), r=['st'], w=['mv'])
                          kb.op('act', lambda e: e.activation(out=rstd[:], in_=mv[:, 1:2], func=AF.Sqrt, bias=epsT[:, 0:1]), r=['mv', 'epsT'], w=['rstd'])
                          kb.op('dve', lambda e: e.reciprocal(out=rstd[:], in_=rstd[:]), r=['rstd'], w=['rstd'])
                          kb.op('dve', lambda e: e.tensor_scalar(out=dst[:], in0=src[:], scalar1=mv[:, 0:1], scalar2=rstd[:, 0:1], op0=ALU.subtract, op1=ALU.mult),
                                r=[srckey, 'mv', 'rstd'], w=[dstkey])

                      for tt in range(NT if need_ctx else NT):
                          if (not need_ctx) and tt < NCT:
                              continue
                          s = tt % 2
                          j = 1 if tt < NCT else 0
                          tsl = slice(tt * 128, (tt + 1) * 128)
                          kb.dma('sp', mg[s][:], ZT[tsl, 896:3968], ('mg', s), r=[K('ZT')], w=[('mg', s)])
                          kb.dma('sp', xr[s][:], X_in[tsl, :], ('xr', s), r=[K('X2w')] if l > 0 else [], w=[('xr', s)])
                          kb.op('act', lambda e, s=s: e.activation(out=sg[:], in_=mg[s][:], func=AF.Sigmoid), r=[('mg', s)], w=['sg'])
                          for n in range(2):
                              ns = slice(n * 512, (n + 1) * 512)
                              kb.mm([lambda e, k=k, ns=ns: e.matmul(pabc[0][:, :], lhsT=yaT[:, k, tsl], rhs=wpa[:, k, ns], start=(k == 0), stop=(k == 1)) for k in range(2)], r=[K('yaT'), 'wpa'], w=[('pabc', 0)])
                              kb.mm([lambda e, k=k, ns=ns: e.matmul(pabc[1][:, :], lhsT=ybT[:, k, tsl], rhs=wpb[:, k, ns], start=(k == 0), stop=(k == 3)) for k in range(4)], r=[K('ybT'), 'wpb'], w=[('pabc', 1)])
                              kb.mm([lambda e, k=k, ns=ns: e.matmul(pabc[2][:, :], lhsT=ycT[:, k, tsl], rhs=wpc[:, k, ns], start=(k == 0), stop=(k == 1)) for k in range(2)], r=[K('ycT'), 'wpc'], w=[('pabc', 2)])
                              kb.op('dve', lambda e, ns=ns: e.tensor_tensor(out=u[:, ns], in0=pabc[0][:, :], in1=sg[:, n * 512:(n + 1) * 512], op=ALU.mult), r=[('pabc', 0), 'sg'], w=['u'])
                              kb.op('dve', lambda e, n=n: e.tensor_tensor(out=tmp[:], in0=pabc[1][:, :], in1=sg[:, D + n * 512:D + (n + 1) * 512], op=ALU.mult), r=[('pabc', 1), 'sg'], w=['tmp'])
                              kb.op('pool', lambda e, ns=ns: e.tensor_tensor(out=u[:, ns], in0=u[:, ns], in1=tmp[:], op=ALU.add), r=['u', 'tmp'], w=['u'])
                              kb.op('dve', lambda e, n=n: e.tensor_tensor(out=tmp[:], in0=pabc[2][:, :], in1=sg[:, 2 * D + n * 512:2 * D + (n + 1) * 512], op=ALU.mult), r=[('pabc', 2), 'sg'], w=['tmp'])
                              kb.op('pool', lambda e, ns=ns: e.tensor_tensor(out=u[:, ns], in0=u[:, ns], in1=tmp[:], op=ALU.add), r=['u', 'tmp'], w=['u'])
                          for hb in range(2):
                              kb.mm([lambda e, k=k, hb=hb: e.transpose(ptp[hb][:, k % 4, :], u[:, k * 128:(k + 1) * 128], ident[:]) for k in range(hb * 4, hb * 4 + 4)], r=['u', 'ident'], w=[('ptp', hb)])
                              kb.op('act', lambda e, hb=hb: e.copy(out=uT[:, hb * 4:hb * 4 + 4, :], in_=ptp[hb][:]), r=[('ptp', hb)], w=['uT'])
                          for n in range(2):
                              kb.mm([lambda e, k=k, n=n: e.matmul(pyl[n][:, :], lhsT=uT[:, k, :], rhs=wo[:, k, n * 512:(n + 1) * 512], start=(k == 0), stop=(k == 7)) for k in range(8)], r=['uT', 'wo'], w=[('pyl', n)])
                              kb.op('dve', lambda e, n=n, j=j: e.tensor_tensor(out=rr[:, n * 512:(n + 1) * 512], in0=pyl[n][:, :], in1=modrow[:, j, 0, n * 512:(n + 1) * 512], op=ALU.mult), r=[('pyl', n), 'modrow'], w=['rr'])
                          kb.op('dve', lambda e, s=s: e.scalar_tensor_tensor(out=rr[:], in0=xr[s][:], scalar=ALPHA, in1=rr[:], op0=ALU.mult, op1=ALU.add), r=[('xr', s), 'rr'], w=['rr'])
                          layer_norm(rr, 'rr', x1, 'x1')
                          kb.op('pool', lambda e: e.tensor_tensor(out=x1[:], in0=x1[:], in1=g1r[:], op=ALU.mult), r=['x1', 'g1r'], w=['x1'])
                          kb.op('pool', lambda e: e.tensor_tensor(out=x1[:], in0=x1[:], in1=b1r[:], op=ALU.add), r=['x1', 'b1r'], w=['x1'])
                          kb.dma('sp', X1[tsl, :], x1[:], 'x1o', r=['x1'], w=[K('X1')])
                          layer_norm(x1, 'x1', xn2, 'xn2')
                          for hb in range(2):
                              kb.mm([lambda e, k=k, hb=hb: e.transpose(ptp[hb][:, k % 4, :], xn2[:, k * 128:(k + 1) * 128], ident[:]) for k in range(hb * 4, hb * 4 + 4)], r=['xn2', 'ident'], w=[('ptp', hb)])
                              for k in range(hb * 4, hb * 4 + 4):
                                  kb.op('act', lambda e, k=k, hb=hb, j=j: e.activation(out=h2f[:, k, :], in_=ptp[hb][:, k % 4, :], func=AF.Identity,
                                                                                       bias=modcol[:, j, 2, k:k + 1], scale=modcol[:, j, 3, k:k + 1]), r=[('ptp', hb), 'modcol'], w=['h2f'])
                          kb.op('pool', lambda e: e.tensor_copy(out=h2b[:], in_=h2f[:]), r=['h2f'], w=['h2b'])
                          kb.dma('sp', H2T[:, :, tsl], h2b[:], 'h2o', r=['h2b'], w=[K('H2T')])
                          kb.mm([lambda e, k=k: e.matmul(prt[:, :], lhsT=h2f[:, k, :], rhs=wr[:, k, :], start=(k == 0), stop=(k == 7)) for k in range(8)], r=['h2f', 'wr'], w=['prt'])
                          kb.op('dve', lambda e, tt=tt: e.tensor_tensor(out=logits[:, tt, :], in0=prt[:, :], in1=brr[:], op=ALU.add), r=['prt', 'brr'], w=['logits'])

              _chk('E')
              t0 = 0 if need_ctx else NCT
              NTE = NT - t0
              with scope() as pes:
                  with scope() as r1:
                      lg = logits[:, t0:NT, :]
                      gmax = sb(r1, [128, NTE], F32, "gmax")
                      gex = sb(r1, [128, NTE, 4], F32, "gex")
                      gsum = sb(r1, [128, NTE], F32, "gsum")
                      ohg = sb(r1, [128, NTE, 4], F32, "ohg")
                      elm = sb(r1, [128, NTE, 4, 8], F32, "elm")
                      els = sb(r1, [128, NTE, 8], F32, "els")
                      m1 = sb(r1, [128, NTE], F32, "m1")
                      m2 = sb(r1, [128, NTE], F32, "m2")
                      oh1 = sb(r1, [128, NTE, 8], F32, "oh1")
                      oh2 = sb(r1, [128, NTE, 8], F32, "oh2")
                      el2 = sb(r1, [128, NTE, 8], F32, "el2")
                      w1 = sb(r1, [128, NTE], F32, "w1")
                      w2 = sb(r1, [128, NTE], F32, "w2")
                      we = sb(r1, [128, NTE, 8], F32, "we")
                      R = 'rt'

                      def v(fn, r=(), w=()):
                          kb.op('dve', fn, r=[R] + list(r), w=[R] + list(w))
                      gl = lg[:, :, 0:4]
                      el = lg[:, :, 4:36].rearrange("p n (g j) -> p n g j", g=4)
                      v(lambda e: e.tensor_reduce(out=gmax[:], in_=gl, axis=AX.X, op=ALU.max), r=['logits'])
                      v(lambda e: e.tensor_tensor(out=gex[:], in0=gl, in1=gmax[:, :].unsqueeze(2).to_broadcast([128, NTE, 4]), op=ALU.subtract), r=['logits'])
                      v(lambda e: e.tensor_tensor(out=ohg[:], in0=gl, in1=gmax[:, :].unsqueeze(2).to_broadcast([128, NTE, 4]), op=ALU.is_ge), r=['logits'])
                      kb.op('act', lambda e: e.activation(out=gex[:], in_=gex[:], func=AF.Exp), r=[R], w=[R])
                      v(lambda e: e.tensor_reduce(out=gsum[:], in_=gex[:], axis=AX.X, op=ALU.add))
                      v(lambda e: e.reciprocal(out=gsum[:], in_=gsum[:]))
                      v(lambda e: e.tensor_tensor(out=elm[:], in0=el, in1=ohg[:].unsqueeze(3).to_broadcast([128, NTE, 4, 8]), op=ALU.mult), r=['logits'])
                      v(lambda e: e.tensor_reduce(out=els[:], in_=elm[:].rearrange("p n g j -> p n j g"), axis=AX.X, op=ALU.add))
                      v(lambda e: e.tensor_reduce(out=m1[:], in_=els[:], axis=AX.X, op=ALU.max))
                      v(lambda e: e.tensor_tensor(out=oh1[:], in0=els[:], in1=m1[:, :].unsqueeze(2).to_broadcast([128, NTE, 8]), op=ALU.is_ge))
                      v(lambda e: e.scalar_tensor_tensor(out=el2[:], in0=oh1[:], scalar=-1e30, in1=els[:], op0=ALU.mult, op1=ALU.add))
                      v(lambda e: e.tensor_reduce(out=m2[:], in_=el2[:], axis=AX.X, op=ALU.max))
                      v(lambda e: e.tensor_tensor(out=oh2[:], in0=el2[:], in1=m2[:, :].unsqueeze(2).to_broadcast([128, NTE, 8]), op=ALU.is_ge))
                      v(lambda e: e.tensor_tensor(out=w2[:], in0=m2[:], in1=m1[:], op=ALU.subtract))
                      kb.op('act', lambda e: e.activation(out=w2[:], in_=w2[:], func=AF.Exp), r=[R], w=[R])
                      v(lambda e: e.tensor_scalar(out=w1[:], in0=w2[:], scalar1=1.0, scalar2=None, op0=ALU.add))
                      v(lambda e: e.reciprocal(out=w1[:], in_=w1[:]))
                      v(lambda e: e.tensor_tensor(out=w2[:], in0=w2[:], in1=w1[:], op=ALU.mult))
                      v(lambda e: e.tensor_tensor(out=w1[:], in0=w1[:], in1=gsum[:], op=ALU.mult))
                      v(lambda e: e.tensor_tensor(out=w2[:], in0=w2[:], in1=gsum[:], op=ALU.mult))
                      v(lambda e: e.tensor_tensor(out=we[:], in0=oh1[:], in1=w1[:, :].unsqueeze(2).to_broadcast([128, NTE, 8]), op=ALU.mult))
                      v(lambda e: e.tensor_tensor(out=oh2[:], in0=oh2[:], in1=w2[:, :].unsqueeze(2).to_broadcast([128, NTE, 8]), op=ALU.mult))
                      v(lambda e: e.tensor_tensor(out=we[:], in0=we[:], in1=oh2[:], op=ALU.add))
                      v(lambda e: e.tensor_tensor(out=wts[:, t0:NT, :].rearrange("p n (g j) -> p n g j", g=4), in0=ohg[:].unsqueeze(3).to_broadcast([128, NTE, 4, 8]),
                                                  in1=we[:].unsqueeze(2).to_broadcast([128, NTE, 4, 8]), op=ALU.mult), w=['wts'])
                  g2r = sb(pes, [128, D], F32, "g2r")
                  b2r = sb(pes, [128, D], F32, "b2r")
                  kb.dma('sp', g2r[:], ln2_g[l].partition_broadcast(128), K('ew2'), w=['g2r'])
                  kb.dma('sp', b2r[:], ln2_b[l].partition_broadcast(128), K('ew2'), w=['b2r'])
                  nh = (NTE + 1) // 2
                  halves = [(t0, t0 + nh), (t0 + nh, NT)]
                  wei = [sb(pes, [128, 8, D], BF16, "wei") for _ in range(2)]
                  weo = [sb(pes, [128, 4, D], BF16, "weo") for _ in range(2)]
                  h2 = sb(pes, [128, 8, nh * 128], BF16, "h2")
                  acc = sb(pes, [128, nh, D], F32, "acc")
                  sgt = sb(pes, [128, 512], F32, "sgt")
                  aT = [sb(pes, [128, 4, 512], BF16, "aT")] * 2
                  xr = [sb(pes, [128, D], F32, "xr2")] * 2
                  rr = sb(pes, [128, D], F32, "rr2")
                  xo = [sb(pes, [128, D], F32, "xo")] * 2
                  st = sb(pes, [128, 2, 6], F32, "st2")
                  mv = sb(pes, [128, 2], F32, "mv2")
                  rstd = sb(pes, [128, 1], F32, "rstd2")
                  pgu = [ps(pes, [128, 512], F32, "pgu") for _ in range(4)]
                  po = [ps(pes, [128, 512], F32, "po") for _ in range(4)]
                  ei = 0
                  for (ha, hb_) in halves:
                      nt_h = hb_ - ha
                      ntok = nt_h * 128
                      kb.dma('sp', h2[:, :, 0:ntok], H2T[:, :, ha * 128:hb_ * 128], 'h2l', r=[K('H2T')], w=['h2'])
                      kb.op('pool', lambda e: e.memset(acc[:].rearrange("p n d -> p (n d)"), 0.0), w=['acc'])
                      groups = [(a, min(a + 512, ntok)) for a in range(0, ntok, 512)]
                      for ex_ in range(NEXP):
                          s = ei % 2
                          ei += 1
                          kb.dma('pool', wei[s][:], w_ei[l, ex_ % nexp_decl].rearrange("(k p) n -> p k n", p=128), ('wei', s), w=[('wei', s)])
                          kb.dma('pool', weo[s][:], w_eo[l, ex_ % nexp_decl].rearrange("(k p) n -> p k n", p=128), ('weo', s), w=[('weo', s)])
                          for gi, (a, b) in enumerate(groups):
                              n = b - a
                              at = aT[gi % 2]
                              for jc in range(4):
                                  pg, pu = pgu[(jc % 2) * 2], pgu[(jc % 2) * 2 + 1]
                                  kb.mm([lambda e, k=k, jc=jc, pg=pg, s=s: e.matmul(pg[:, 0:n], lhsT=wei[s][:, k, jc * 128:(jc + 1) * 128], rhs=h2[:, k, a:b], start=(k == 0), stop=(k == 7)) for k in range(8)],
                                        r=[('wei', s), 'h2'], w=[('pgu', (jc % 2) * 2)])
                                  kb.mm([lambda e, k=k, jc=jc, pu=pu, s=s: e.matmul(pu[:, 0:n], lhsT=wei[s][:, k, 512 + jc * 128:512 + (jc + 1) * 128], rhs=h2[:, k, a:b], start=(k == 0), stop=(k == 7)) for k in range(8)],
                                        r=[('wei', s), 'h2'], w=[('pgu', (jc % 2) * 2 + 1)])
                                  kb.op('act', lambda e, pg=pg: e.activation(out=sgt[:, 0:n], in_=pg[:, 0:n], func=AF.Silu), r=[('pgu', (jc % 2) * 2)], w=['sgt'])
                                  kb.op('dve', lambda e, pu=pu, jc=jc, at=at: e.tensor_tensor(out=at[:, jc, 0:n], in0=pu[:, 0:n], in1=sgt[:, 0:n], op=ALU.mult),
                                        r=[('pgu', (jc % 2) * 2 + 1), 'sgt'], w=[('aT', 0)])
                              for ti in range(a // 128, b // 128):
                                  tloc = ti * 128 - a
                                  for n2 in range(2):
                                      pb_ = po[(ti % 2) * 2 + n2]
                                      kb.mm([lambda e, k=k, pb_=pb_, tloc=tloc, n2=n2, at=at, s=s: e.matmul(pb_[:, :], lhsT=at[:, k, tloc:tloc + 128], rhs=weo[s][:, k, n2 * 512:(n2 + 1) * 512], start=(k == 0), stop=(k == 3)) for k in range(4)],
                                            r=[('aT', 0), ('weo', s)], w=[('po', (ti % 2) * 2 + n2)])
                                      kb.op('dve', lambda e, pb_=pb_, ti=ti, n2=n2, ex_=ex_: e.scalar_tensor_tensor(
                                          out=acc[:, ti, n2 * 512:(n2 + 1) * 512], in0=pb_[:, :], scalar=wts[:, ha + ti, ex_:ex_ + 1], in1=acc[:, ti, n2 * 512:(n2 + 1) * 512], op0=ALU.mult, op1=ALU.add),
                                          r=[('po', (ti % 2) * 2 + n2), 'wts', 'acc'], w=['acc'])
                      for ti in range(nt_h):
                          tt = ha + ti
                          s = tt % 2
                          j = 1 if tt < NCT else 0
                          tsl = slice(tt * 128, (tt + 1) * 128)
                          kb.dma('sp', xr[s][:], X1[tsl, :], ('xr2', 0), r=[K('X1')], w=[('xr2', 0)])
                          kb.op('pool', lambda e, ti=ti, j=j: e.tensor_tensor(out=rr[:], in0=acc[:, ti, :], in1=modrow[:, j, 1, :], op=ALU.mult), r=['acc', 'modrow'], w=['rr2'])
                          kb.op('dve', lambda e, s=s: e.scalar_tensor_tensor(out=rr[:], in0=xr[s][:], scalar=ALPHA, in1=rr[:], op0=ALU.mult, op1=ALU.add), r=[('xr2', 0), 'rr2'], w=['rr2'])
                          for h in range(2):
                              kb.op('dve', lambda e, h=h: e.bn_stats(out=st[:, h, :], in_=rr[:, h * 512:(h + 1) * 512]), r=['rr2'], w=['st2'])
                          kb.op('dve', lambda e: e.bn_aggr(out=mv[:], in_=st[:].rearrange("p a b -> p (a b)")), r=['st2'], w=['mv2'])
                          kb.op('act', lambda e: e.activation(out=rstd[:], in_=mv[:, 1:2], func=AF.Sqrt, bias=epsT[:, 0:1]), r=['mv2', 'epsT'], w=['rstd2'])
                          kb.op('dve', lambda e: e.reciprocal(out=rstd[:], in_=rstd[:]), r=['rstd2'], w=['rstd2'])
                          kb.op('dve', lambda e, s=s: e.tensor_scalar(out=xo[s][:], in0=rr[:], scalar1=mv[:, 0:1], scalar2=rstd[:, 0:1], op0=ALU.subtract, op1=ALU.mult),
                                r=['rr2', 'mv2', 'rstd2'], w=[('xo', 0)])
                          kb.op('pool', lambda e, s=s: e.tensor_tensor(out=xo[s][:], in0=xo[s][:], in1=g2r[:], op=ALU.mult), r=[('xo', 0), 'g2r'], w=[('xo', 0)])
                          kb.op('pool', lambda e, s=s: e.tensor_tensor(out=xo[s][:], in0=xo[s][:], in1=b2r[:], op=ALU.add), r=[('xo', 0), 'b2r'], w=[('xo', 0)])
                          if l == DEPTH - 1:
                              kb.dma('sp', y_out[(tt - NCT) * 128:(tt - NCT + 1) * 128, :], xo[s][:], ('xoo', 0), r=[('xo', 0)], w=['yout'])
                          else:
                              kb.dma('sp', X_out[tsl, :], xo[s][:], ('xoo', 0), r=[('xo', 0)], w=[('X2w', l + 1)])

        except _Stop:
            pass
        kb.mute = False
        kb.flush('sp')
    return nc


_PF, _PT = _perm_cols()


def _prep_shared(inp, NLT):
    f32 = np.float32
    w_in = np.asarray(inp['w_in'], f32)
    sh = {
        'w_ada': np.ascontiguousarray(inp['w_ada'], f32),
        'b_ada': np.ascontiguousarray(inp['b_ada'], f32),
        'w_f': np.ascontiguousarray(w_in[:, :, _PF]),
        'w_t': np.ascontiguousarray(w_in[:, :, _PT]),
        'conv_w': np.ascontiguousarray(np.asarray(inp['conv_w'], f32).reshape(DEPTH, 3, 2, 128).transpose(0, 3, 2, 1)),
        'sink': np.ascontiguousarray(inp['attn_sink'], f32),
        'gate_b': np.ascontiguousarray(inp['mlstm_gate_b'], f32),
        'norm_w': np.ascontiguousarray(inp['mlstm_norm_w'], f32),
        'w_pa': np.ascontiguousarray(inp['w_proj_a'], f32),
        'w_pb': np.ascontiguousarray(inp['w_proj_b'], f32),
        'w_pc': np.ascontiguousarray(inp['w_proj_c'], f32),
        'w_o': np.ascontiguousarray(inp['w_out'], f32),
        'ln1_g': np.ascontiguousarray(inp['ln1_g'], f32),
        'ln1_b': np.ascontiguousarray(inp['ln1_b'], f32),
        'ln2_g': np.ascontiguousarray(inp['ln2_g'], f32),
        'ln2_b': np.ascontiguousarray(inp['ln2_b'], f32),
        'w_r': np.ascontiguousarray(np.concatenate([inp['w_route_group'], inp['w_route_expert']], axis=-1), f32),
        'b_r': np.ascontiguousarray(np.concatenate([inp['b_route_group'], inp['b_route_expert']], axis=-1), f32),
        'w_ei': np.ascontiguousarray(inp['w_expert_in'], f32),
        'w_eo': np.ascontiguousarray(inp['w_expert_out'], f32),
    }
    for k, v in _consts(NLT).items():
        sh['c_' + k] = v
    return sh


def _prep_core(inp, b):
    f32 = np.float32
    m = {}
    m['xin'] = np.ascontiguousarray(np.concatenate([inp['ctx'][b], inp['x'][b]], axis=0), f32)
    cs = np.stack([np.asarray(inp['c'][b], f32), np.asarray(inp['c_ctx'], f32)], axis=-1)
    m['cs'] = np.ascontiguousarray(cs.reshape(8, 128, 2).transpose(1, 0, 2))
    return m


def kernel(**inputs):
    inp = {k: np.asarray(v) for k, v in inputs.items()}
    B, L, _ = inp['x'].shape
    NLT = L // 128
    nc = build(NLT)
    sh = _prep_shared(inp, NLT)
    in_maps = []
    for b in range(B):
        m = dict(sh)
        m.update(_prep_core(inp, b))
        in_maps.append(m)
    res = run_bass_kernel_spmd(nc, in_maps, core_ids=list(range(B)))
    return np.stack([np.asarray(r['y'], np.float32) for r in res.results], axis=0)
```
